# Optimizing a Trainium2 kernel written in Bass

```python
import jax
import jax.numpy as jnp
from jax import lax
import numpy as np

D_MODEL = 2048
BATCH = 4
SEQ = 8192
DEPTH = 2

GRID_W = 64
CTX_LEN = 256
HEAD_DIM = 128
N_HEADS_TOTAL = D_MODEL // HEAD_DIM
N_HEADS_MLA = N_HEADS_TOTAL // 2
N_HEADS_NAT = N_HEADS_TOTAL // 4
N_HEADS_RET = N_HEADS_TOTAL - N_HEADS_MLA - N_HEADS_NAT
MLA_NOPE_DIM = HEAD_DIM
MLA_ROPE_DIM = 64
MLA_V_DIM = HEAD_DIM
MLA_Q_LORA = D_MODEL // 4
MLA_KV_LORA = D_MODEL // 8
MLA_SCALE = (MLA_NOPE_DIM + MLA_ROPE_DIM) ** -0.5
NAT_ROWS = 8
NAT_COLS = 16
RET_CHUNK = 128
MLA_W = N_HEADS_MLA * MLA_V_DIM
NAT_W = N_HEADS_NAT * HEAD_DIM
RET_W = N_HEADS_RET * HEAD_DIM
D_MIX = MLA_W + NAT_W + RET_W
IN_SPLITS = (MLA_Q_LORA, MLA_KV_LORA, MLA_ROPE_DIM, NAT_W, NAT_W, NAT_W, RET_W, RET_W, RET_W, RET_W, RET_W)
IN_COLS = sum(IN_SPLITS)
N_EXPERTS = 16
EXPERT_FF = D_MODEL // 2
EC_CAPACITY = 2
Q_BLOCK = 128
ROPE_BASE = 10000.0
EPS = 1e-6

kernel_name = 'hybrid_mla_nat_retention_ecmoe_dit'


def _rmsnorm(x, g):
    xf = x.astype(jnp.float32)
    y = xf * lax.rsqrt(jnp.mean(xf * xf, axis=-1, keepdims=True) + EPS)
    return (y * g.astype(jnp.float32)).astype(x.dtype)


def _heads(t, n_heads):
    return t.reshape(t.shape[:-1] + (n_heads, t.shape[-1] // n_heads))


def _split_cols(p):
    return jnp.split(p, np.cumsum(IN_SPLITS)[:-1].tolist(), axis=-1)


def _modulate(h, shift, scale):
    return h * (1 + scale) + shift


def _rope_tables(n):
    t = jnp.arange(n)
    n_freq = MLA_ROPE_DIM // 4
    inv = ROPE_BASE ** (-jnp.arange(n_freq, dtype=jnp.float32) / n_freq)
    ang_r = (t // GRID_W).astype(jnp.float32)[:, None] * inv
    ang_c = (t % GRID_W).astype(jnp.float32)[:, None] * inv
    return (jnp.cos(ang_r)[:, None], jnp.sin(ang_r)[:, None], jnp.cos(ang_c)[:, None], jnp.sin(ang_c)[:, None])


def _rot_half(x, cos, sin):
    x1, x2 = jnp.split(x, 2, axis=-1)
    return jnp.concatenate([x1 * cos - x2 * sin, x1 * sin + x2 * cos], axis=-1)


def _axial_rope(x, tables):
    cr, sr, cc, sc = tables
    xr, xc = jnp.split(x.astype(jnp.float32), 2, axis=-1)
    return jnp.concatenate([_rot_half(xr, cr, sr), _rot_half(xc, cc, sc)], axis=-1).astype(x.dtype)


def _block_attn(q, k, v, scale):
    b, sq, h, dq = q.shape
    qb = q.reshape(b, sq // Q_BLOCK, Q_BLOCK, h, dq).transpose(1, 0, 2, 3, 4)

    def one_block(q_blk):
        s = jnp.einsum('bqhc,bkc->bhqk', q_blk, k).astype(jnp.float32) * scale
        p = jax.nn.softmax(s, axis=-1).astype(v.dtype)
        return jnp.einsum('bhqk,bkc->bqhc', p, v)

    o = lax.map(one_block, qb)
    return o.transpose(1, 0, 2, 3, 4).reshape(b, sq, h, v.shape[-1])


def _mla_keys(ckv_raw, kr_raw, kv_g, tables):
    ckv = _rmsnorm(ckv_raw, kv_g)
    kr = kr_raw if tables is None else _axial_rope(kr_raw[:, :, None], tables)[:, :, 0]
    return jnp.concatenate([ckv, kr], axis=-1), ckv


def _mla_queries(cq_raw, q_g, w_uq, w_uk, tables):
    q = _heads(_rmsnorm(cq_raw, q_g) @ w_uq, N_HEADS_MLA)
    q_nope, q_rope = jnp.split(q, [MLA_NOPE_DIM], axis=-1)
    if tables is not None:
        q_rope = _axial_rope(q_rope, tables)
    q_abs = jnp.einsum('bnhd,chd->bnhc', q_nope, w_uk)
    return jnp.concatenate([q_abs, q_rope], axis=-1)


def _mla_out(o_lat, w_uv):
    o = jnp.einsum('bnhc,chd->bnhd', o_lat, w_uv)
    return o.reshape(o.shape[0], o.shape[1], -1)


def _nat_latent(q, k, v, k_ctx, v_ctx, rpb):
    b, n, h, dh = q.shape
    rows = n // GRID_W
    kr = min(NAT_ROWS, rows)
    scale = dh ** -0.5
    qg = q.reshape(b, rows, GRID_W, h, dh).transpose(1, 0, 2, 3, 4)
    kg = k.reshape(b, rows, GRID_W, h, dh)
    vg = v.reshape(b, rows, GRID_W, h, dh)
    col = jnp.arange(GRID_W)
    col_idx = jnp.clip(col - NAT_COLS // 2, 0, GRID_W - NAT_COLS)[:, None] + jnp.arange(NAT_COLS)
    bias_c = rpb[:, :, col_idx - col[:, None] + NAT_COLS - 1]

    def row_block(args):
        r, q_r = args
        r0 = jnp.clip(r - kr // 2, 0, rows - kr)
        k_win = lax.dynamic_slice_in_dim(kg, r0, kr, axis=1)[:, :, col_idx]
        v_win = lax.dynamic_slice_in_dim(vg, r0, kr, axis=1)[:, :, col_idx]
        dr = r0 + jnp.arange(kr) - r + NAT_ROWS - 1
        bias = jnp.take(bias_c, dr, axis=1).transpose(0, 2, 1, 3)
        s_win = jnp.einsum('bqhd,brqchd->bhqrc', q_r, k_win).astype(jnp.float32) * scale + bias
        s_ctx = jnp.einsum('bqhd,bkhd->bhqk', q_r, k_ctx).astype(jnp.float32) * scale
        s = jnp.concatenate([s_win.reshape(b, h, GRID_W, kr * NAT_COLS), s_ctx], axis=-1)
        p = jax.nn.softmax(s, axis=-1).astype(v.dtype)
        p_win = p[..., :kr * NAT_COLS].reshape(b, h, GRID_W, kr, NAT_COLS)
        return (jnp.einsum('bhqrc,brqchd->bqhd', p_win, v_win)
                + jnp.einsum('bhqk,bkhd->bqhd', p[..., kr * NAT_COLS:], v_ctx))

    o = lax.map(row_block, (jnp.arange(rows), qg))
    return o.transpose(1, 0, 2, 3, 4).reshape(b, n, h * dh)


def _nat_context(q, k, v):
    b, L, h, dh = q.shape
    s = jnp.einsum('bqhd,bkhd->bhqk', q, k).astype(jnp.float32) * dh ** -0.5
    p = jax.nn.softmax(s, axis=-1).astype(v.dtype)
    return jnp.einsum('bhqk,bkhd->bqhd', p, v).reshape(b, L, h * dh)


def _retention_dir(q, k, v, log_g, s0):
    b, h, L, dk = k.shape
    dv = v.shape[-1]
    n = L // RET_CHUNK
    kc = k.reshape(b, h, n, RET_CHUNK, dk)
    vc = v.reshape(b, h, n, RET_CHUNK, dv)
    pos = jnp.arange(RET_CHUNK, dtype=jnp.float32)
    w_k = jnp.exp(log_g[:, None] * (RET_CHUNK - 1 - pos))
    kv = jnp.einsum('bhncd,hc,bhnce->nbhde', kc, w_k, vc)
    g_chunk = jnp.exp(log_g * RET_CHUNK)[None, :, None, None]

    def step(s, kv_n):
        return g_chunk * s + kv_n, s

    s_final, s_prev = lax.scan(step, s0, kv)
    if q is None:
        return None, s_final
    qc = q.reshape(b, h, n, RET_CHUNK, dk)
    rel = pos[:, None] - pos[None, :]
    dmat = jnp.where(rel >= 0, jnp.exp(log_g[:, None, None] * jnp.maximum(rel, 0.0)), 0.0)
    scores = jnp.einsum('bhncd,bhnsd->bhncs', qc, kc) * dmat[:, None]
    o_inner = jnp.einsum('bhncs,bhnse->bhnce', scores, vc)
    w_q = jnp.exp(log_g[:, None] * (pos + 1.0))
    o_cross = jnp.einsum('bhncd,hc,nbhde->bhnce', qc, w_q, s_prev)
    return (o_inner + o_cross).reshape(b, h, L, dv), s_final


def _head_groupnorm(o):
    mu = jnp.mean(o, axis=-1, keepdims=True)
    var = jnp.mean(jnp.square(o - mu), axis=-1, keepdims=True)
    return (o - mu) * lax.rsqrt(var + EPS)


def _ret_merge(o_f, o_b, g_f, g_b):
    b, h, L, dv = o_f.shape

    def to_tok(o):
        return _head_groupnorm(o).transpose(0, 2, 1, 3).reshape(b, L, h * dv)

    y = jax.nn.silu(g_f.astype(jnp.float32)) * to_tok(o_f) + jax.nn.silu(g_b.astype(jnp.float32)) * to_tok(o_b)
    return y.astype(g_f.dtype)


def _retention(cols_c, cols_l, logit_f, logit_b, ctx_out):
    def prep(t):
        return _heads(t, N_HEADS_RET).transpose(0, 2, 1, 3).astype(jnp.float32)

    k_scale = HEAD_DIM ** -0.5
    qc, kc, vc = prep(cols_c[0]), prep(cols_c[1]) * k_scale, prep(cols_c[2])
    ql, kl, vl = prep(cols_l[0]), prep(cols_l[1]) * k_scale, prep(cols_l[2])
    lg_f = -jax.nn.softplus(-logit_f.astype(jnp.float32))
    lg_b = -jax.nn.softplus(-logit_b.astype(jnp.float32))
    s0 = jnp.zeros(kc.shape[:2] + (HEAD_DIM, HEAD_DIM), jnp.float32)

    def flip(t):
        return t[:, :, ::-1]

    o_cf, s_cf = _retention_dir(qc if ctx_out else None, kc, vc, lg_f, s0)
    o_cb, s_cb = _retention_dir(flip(qc) if ctx_out else None, flip(kc), flip(vc), lg_b, s0)
    o_lf, _ = _retention_dir(ql, kl, vl, lg_f, s_cf)
    o_lb, _ = _retention_dir(flip(ql), flip(kl), flip(vl), lg_b, s_cb)
    y_l = _ret_merge(o_lf, flip(o_lb), cols_l[3], cols_l[4])
    y_c = _ret_merge(o_cf, flip(o_cb), cols_c[3], cols_c[4]) if ctx_out else None
    return y_c, y_l


def _merge_groups(ys, g):
    gs = jnp.split(g, [MLA_W, MLA_W + NAT_W])
    return jnp.concatenate([_rmsnorm(y, gi) for y, gi in zip(ys, gs)], axis=-1)


def _ec_moe(h, w_router, w_gate, w_up, w_down):
    b, n, d = h.shape
    cap = EC_CAPACITY * n // N_EXPERTS
    aff = jax.nn.softmax((h @ w_router).astype(jnp.float32), axis=-1)
    gate, idx = lax.top_k(jnp.swapaxes(aff, 1, 2), cap)
    xs = jax.vmap(lambda hb, ib: hb[ib])(h, idx)
    a = jnp.einsum('becd,edf->becf', xs, w_gate)
    u = jnp.einsum('becd,edf->becf', xs, w_up)
    y = jnp.einsum('becf,efd->becd', jax.nn.silu(a) * u, w_down) * gate[..., None].astype(h.dtype)
    return jax.vmap(lambda ib, yb: jnp.zeros((n, d), h.dtype).at[ib.reshape(-1)].add(yb.reshape(-1, d)))(idx, y)


def _layer(x_l, x_c, c, c_ctx, lp, last):
    mod_l = jax.nn.silu(c) @ lp['w_mod'] + lp['b_mod']
    mod_c = jax.nn.silu(c_ctx) @ lp['w_mod'] + lp['b_mod']
    sh1_l, sc1_l, g1_l, sh2_l, sc2_l, g2_l = jnp.split(mod_l[:, None, :], 6, axis=-1)
    sh1_c, sc1_c, g1_c, sh2_c, sc2_c, g2_c = jnp.split(mod_c, 6, axis=-1)
    p_l = _split_cols(_modulate(_rmsnorm(x_l, lp['norm1_g']), sh1_l, sc1_l) @ lp['w_in'])
    p_c = _split_cols(_modulate(_rmsnorm(x_c, lp['norm1_g']), sh1_c, sc1_c) @ lp['w_in'])
    tables = _rope_tables(x_l.shape[1])

    k_c, v_c = _mla_keys(p_c[1], p_c[2], lp['mla_kv_norm_g'], None)
    k_l, v_l = _mla_keys(p_l[1], p_l[2], lp['mla_kv_norm_g'], tables)
    q_l = _mla_queries(p_l[0], lp['mla_q_norm_g'], lp['mla_w_uq'], lp['mla_w_uk'], tables)
    a_l = _mla_out(_block_attn(q_l, jnp.concatenate([k_c, k_l], axis=1), jnp.concatenate([v_c, v_l], axis=1), MLA_SCALE), lp['mla_w_uv'])

    nk_c, nv_c = _heads(p_c[4], N_HEADS_NAT), _heads(p_c[5], N_HEADS_NAT)
    b_l = _nat_latent(_heads(p_l[3], N_HEADS_NAT), _heads(p_l[4], N_HEADS_NAT), _heads(p_l[5], N_HEADS_NAT), nk_c, nv_c, lp['nat_rpb'])

    r_c, r_l = _retention(p_c[6:], p_l[6:], lp['ret_decay_f'], lp['ret_decay_b'], not last)

    x_l = x_l + g1_l * (_merge_groups((a_l, b_l, r_l), lp['out_norm_g']) @ lp['w_out'])
    x_l = x_l + g2_l * _ec_moe(_modulate(_rmsnorm(x_l, lp['norm2_g']), sh2_l, sc2_l),
                               lp['w_router'], lp['w_gate'], lp['w_up'], lp['w_down'])
    if last:
        return x_l, None

    q_c = _mla_queries(p_c[0], lp['mla_q_norm_g'], lp['mla_w_uq'], lp['mla_w_uk'], None)
    a_c = _mla_out(_block_attn(q_c, k_c, v_c, MLA_SCALE), lp['mla_w_uv'])
    b_c = _nat_context(_heads(p_c[3], N_HEADS_NAT), nk_c, nv_c)
    x_c = x_c + g1_c * (_merge_groups((a_c, b_c, r_c), lp['out_norm_g']) @ lp['w_out'])
    x_c = x_c + g2_c * _ec_moe(_modulate(_rmsnorm(x_c, lp['norm2_g']), sh2_c, sc2_c),
                               lp['w_router'], lp['w_gate'], lp['w_up'], lp['w_down'])
    return x_l, x_c


def setup_inputs(seed: int = 0) -> dict:
    key = jax.random.key(seed)
    ks = jax.random.split(key, 24)
    f32 = jnp.float32

    def nrm(k, shape, s):
        return jax.random.normal(k, shape, f32) * s

    def gain(k, shape):
        return 1.0 + 0.02 * jax.random.normal(k, shape, f32)

    base_logit = jnp.log(2.0 ** (5.0 + jnp.arange(N_HEADS_RET, dtype=f32)) - 1.0)
    return {
        'x': nrm(ks[0], (BATCH, SEQ, D_MODEL), 1.0),
        'c': nrm(ks[1], (BATCH, D_MODEL), 1.0),
        'ctx': nrm(ks[2], (BATCH, CTX_LEN, D_MODEL), 1.0),
        'c_ctx': nrm(ks[3], (D_MODEL,), 1.0),
        'w_mod': nrm(ks[4], (DEPTH, D_MODEL, 6 * D_MODEL), D_MODEL ** -0.5),
        'b_mod': nrm(ks[5], (DEPTH, 6 * D_MODEL), 0.01),
        'norm1_g': gain(ks[6], (DEPTH, D_MODEL)),
        'w_in': nrm(ks[7], (DEPTH, D_MODEL, IN_COLS), D_MODEL ** -0.5),
        'mla_q_norm_g': gain(ks[8], (DEPTH, MLA_Q_LORA)),
        'mla_kv_norm_g': gain(ks[9], (DEPTH, MLA_KV_LORA)),
        'mla_w_uq': nrm(ks[10], (DEPTH, MLA_Q_LORA, N_HEADS_MLA * (MLA_NOPE_DIM + MLA_ROPE_DIM)), MLA_Q_LORA ** -0.5),
        'mla_w_uk': nrm(ks[11], (DEPTH, MLA_KV_LORA, N_HEADS_MLA, MLA_NOPE_DIM), MLA_KV_LORA ** -0.5),
        'mla_w_uv': nrm(ks[12], (DEPTH, MLA_KV_LORA, N_HEADS_MLA, MLA_V_DIM), MLA_KV_LORA ** -0.5),
        'nat_rpb': nrm(ks[13], (DEPTH, N_HEADS_NAT, 2 * NAT_ROWS - 1, 2 * NAT_COLS - 1), 0.1),
        'ret_decay_f': base_logit + nrm(ks[14], (DEPTH, N_HEADS_RET), 0.1),
        'ret_decay_b': base_logit + nrm(ks[15], (DEPTH, N_HEADS_RET), 0.1),
        'out_norm_g': gain(ks[16], (DEPTH, D_MIX)),
        'w_out': nrm(ks[17], (DEPTH, D_MIX, D_MODEL), D_MIX ** -0.5),
        'norm2_g': gain(ks[18], (DEPTH, D_MODEL)),
        'w_router': nrm(ks[19], (DEPTH, D_MODEL, N_EXPERTS), D_MODEL ** -0.5),
        'w_gate': nrm(ks[20], (DEPTH, N_EXPERTS, D_MODEL, EXPERT_FF), D_MODEL ** -0.5),
        'w_up': nrm(ks[21], (DEPTH, N_EXPERTS, D_MODEL, EXPERT_FF), D_MODEL ** -0.5),
        'w_down': nrm(ks[22], (DEPTH, N_EXPERTS, EXPERT_FF, D_MODEL), EXPERT_FF ** -0.5),
        'final_norm_g': gain(ks[23], (D_MODEL,)),
    }


def reference(x, c, ctx, c_ctx, w_mod, b_mod, norm1_g, w_in, mla_q_norm_g, mla_kv_norm_g, mla_w_uq, mla_w_uk,
              mla_w_uv, nat_rpb, ret_decay_f, ret_decay_b, out_norm_g, w_out, norm2_g, w_router, w_gate, w_up,
              w_down, final_norm_g):
    x_l, x_c = x, ctx
    for i in range(DEPTH):
        lp = {
            'w_mod': w_mod[i], 'b_mod': b_mod[i], 'norm1_g': norm1_g[i], 'w_in': w_in[i],
            'mla_q_norm_g': mla_q_norm_g[i], 'mla_kv_norm_g': mla_kv_norm_g[i], 'mla_w_uq': mla_w_uq[i],
            'mla_w_uk': mla_w_uk[i], 'mla_w_uv': mla_w_uv[i], 'nat_rpb': nat_rpb[i],
            'ret_decay_f': ret_decay_f[i], 'ret_decay_b': ret_decay_b[i], 'out_norm_g': out_norm_g[i],
            'w_out': w_out[i], 'norm2_g': norm2_g[i], 'w_router': w_router[i], 'w_gate': w_gate[i],
            'w_up': w_up[i], 'w_down': w_down[i],
        }
        x_l, x_c = _layer(x_l, x_c, c, c_ctx, lp, i == DEPTH - 1)
    return _rmsnorm(x_l, final_norm_g)
```

```python
import numpy as np
import concourse.bass as bass
import concourse.mybir as mybir
from concourse.bass_utils import run_bass_kernel_spmd

F32 = mybir.dt.float32
BF16 = mybir.dt.bfloat16
I32 = mybir.dt.int32
AF = mybir.ActivationFunctionType
ALU = mybir.AluOpType
AX = mybir.AxisListType

ENGS = ("pe", "act", "dve", "pool", "sp")
NDSEM = 12
EPS = 1e-6


def _box(ap):
    a = ap.ap
    off = int(ap.offset)
    sp = str(ap.space)
    if sp == "DRAM":
        hi = off
        for st, cn in a:
            hi += (cn - 1) * abs(st)
        return (0, 1, off, hi + 1)
    pstep = a[0][0]
    if pstep == 0:
        raise ValueError("partition-broadcast AP needs explicit box")
    p0 = off // pstep
    f0 = off % pstep
    hi = f0
    for st, cn in a[1:]:
        hi += (cn - 1) * abs(st)
    return (p0, p0 + a[0][1], f0, hi + 1)


def _ovl(b1, b2):
    return b1[0] < b2[1] and b2[0] < b1[1] and b1[2] < b2[3] and b2[2] < b1[3]


def _contains(b1, b2):
    return b1[0] <= b2[0] and b1[1] >= b2[1] and b1[2] <= b2[2] and b1[3] >= b2[3]


class Prog:
    def __init__(self):
        self.nc = bass.Bass("TRN2", target_bir_lowering=False)
        self.items = {e: [] for e in ENGS}
        self.cnt = {e: 0 for e in ENGS}
        self.dcnt = {e: 0 for e in ENGS}
        self.track = {}
        self._ctx = []
        self.n_inst = 0
        self.n_wait = {}
        self._scopes = []
        self._eps = self.sb("eps_c", [128, 1], F32)
        self.memset(self._eps[:], EPS)

    def dram(self, name, shape, dt, kind="Internal"):
        return self.nc.dram_tensor(name, list(shape), dt, kind=kind).ap()

    def sb(self, name, shape, dt):
        self._uid = getattr(self, "_uid", 0) + 1
        return self._alloc(lambda: self.nc.sbuf_tensor(f"{name}_{self._uid}", list(shape), dt))

    def ps(self, name, shape, dt=F32):
        self._uid = getattr(self, "_uid", 0) + 1
        return self._alloc(lambda: self.nc.psum_tensor(f"{name}_{self._uid}", list(shape), dt))

    def _alloc(self, mk):
        g = mk()
        t = g.__enter__()
        if self._scopes:
            self._scopes[-1].append(g)
        else:
            self._ctx.append(g)
        return t

    _scopes = []

    def scope(self):
        prog = self

        class _S:
            def __enter__(s2):
                prog._scopes = prog._scopes + [[]]
                return s2

            def __exit__(s2, *a):
                prog.barrier()
                gs = prog._scopes[-1]
                prog._scopes = prog._scopes[:-1]
                for g in reversed(gs):
                    g.__exit__(None, None, None)
                return False

        return _S()

    def barrier(self):
        deps = {}
        for e in ENGS:
            if self.cnt[e]:
                deps[e] = self.cnt[e]
            for s in range(min(NDSEM, self.dcnt[e])):
                uses = (self.dcnt[e] - 1 - s) // NDSEM + 1
                deps[("d", e, s)] = 16 * uses
        for e in ENGS:
            self.items[e].append((dict(deps), None, None))
        self.track = {}

    def _deps(self, reads, writes, tok_self):
        deps = {}

        def add(tok):
            k, v = tok
            if deps.get(k, 0) < v:
                deps[k] = v

        for ap in reads:
            bx = ap if isinstance(ap, tuple) else (ap.name, _box(ap))
            nm, b = bx
            tr = self.track.setdefault(nm, {"w": [], "r": {}})
            for (wb, tok) in tr["w"]:
                if _ovl(wb, b):
                    add(tok)
        for ap in writes:
            bx = ap if isinstance(ap, tuple) else (ap.name, _box(ap))
            nm, b = bx
            tr = self.track.setdefault(nm, {"w": [], "r": {}})
            for (wb, tok) in tr["w"]:
                if _ovl(wb, b):
                    add(tok)
            for (rb, rk), rv in tr["r"].items():
                if _ovl(rb, b):
                    add((rk, rv))
        for ap in reads:
            bx = ap if isinstance(ap, tuple) else (ap.name, _box(ap))
            nm, b = bx
            tr = self.track[nm]
            key = (b, tok_self[0])
            tr["r"][key] = tok_self[1]
        for ap in writes:
            bx = ap if isinstance(ap, tuple) else (ap.name, _box(ap))
            nm, b = bx
            tr = self.track[nm]
            tr["w"] = [(wb, t) for (wb, t) in tr["w"] if not _contains(b, wb)]
            tr["r"] = {k: v for k, v in tr["r"].items() if not _contains(b, k[0])}
            tr["w"].append((b, tok_self))
        return deps

    def op(self, eng, fn, reads=(), writes=(), pe_acc=False):
        self.cnt[eng] += 1
        tok = (eng, self.cnt[eng])
        deps = self._deps(reads, writes, tok)
        if eng == "pe":
            deps.pop("pe", None)
        self.items[eng].append((deps, fn, ("c", eng)))
        self.n_inst += 1

    def dma(self, out, in_, eng="sp", reads=None, writes=None, **kw):
        j = self.dcnt[eng]
        self.dcnt[eng] += 1
        slot = j % NDSEM
        use = j // NDSEM + 1
        key = ("d", eng, slot)
        tok = (key, 16 * use)
        deps = self._deps([in_] if reads is None else reads,
                          [out] if writes is None else writes, tok)
        if use > 1:
            if deps.get(key, 0) < 16 * (use - 1):
                deps[key] = 16 * (use - 1)

        def fn(e, out=out, in_=in_, kw=kw):
            return e.dma_start(out=out, in_=in_, **kw)

        self.items[eng].append((deps, fn, key))
        self.n_inst += 1

    def bc_reg(self, eng, val):
        if not hasattr(self, "_bcregs"):
            self._bcregs = {}
        if val not in self._bcregs:
            self._bcregs[val] = eng.to_reg(val)
        return self._bcregs[val]

    def dma_custom(self, eng, fn, reads, writes):
        j = self.dcnt[eng]
        self.dcnt[eng] += 1
        slot = j % NDSEM
        use = j // NDSEM + 1
        key = ("d", eng, slot)
        tok = (key, 16 * use)
        deps = self._deps(reads, writes, tok)
        if use > 1 and deps.get(key, 0) < 16 * (use - 1):
            deps[key] = 16 * (use - 1)
        self.items[eng].append((deps, fn, key))
        self.n_inst += 1

    def finish(self, final_waits=True):
        nc = self.nc
        sems = {}
        sem_ctx = []

        def getsem(k):
            if k not in sems:
                g = nc.semaphore("s_" + "_".join(str(x) for x in (k if isinstance(k, tuple) else (k,))))
                sems[k] = g.__enter__()
                sem_ctx.append(g)
            return sems[k]

        for e in ENGS:
            getsem(e)
        for e in ENGS:
            for s in range(min(NDSEM, self.dcnt[e])):
                getsem(("d", e, s))
        final = {}
        for e in ENGS:
            if self.cnt[e]:
                final[e] = self.cnt[e]
            for s in range(min(NDSEM, self.dcnt[e])):
                uses = (self.dcnt[e] - 1 - s) // NDSEM + 1
                final[("d", e, s)] = 16 * uses
        items = self.items
        blk = nc.Block()
        block = blk.__enter__()

        def mk(ename):
            def body(eng):
                seen = {}
                for deps, fn, inc in items[ename]:
                    for k, v in deps.items():
                        if seen.get(k, 0) < v:
                            eng.wait_ge(sems[k], v)
                            seen[k] = v
                            self.n_wait[ename] = self.n_wait.get(ename, 0) + 1
                    if fn is None:
                        continue
                    ins = fn(eng)
                    if inc[0] == "c":
                        ins.then_inc(sems[inc[1]], 1)
                    else:
                        ins.then_inc(sems[inc], 16)
                if ename == "sp" and final_waits:
                    for k, v in final.items():
                        if seen.get(k, 0) < v:
                            eng.wait_ge(sems[k], v)
            return body

        block.tensor(mk("pe"))
        block.scalar(mk("act"))
        block.vector(mk("dve"))
        block.gpsimd(mk("pool"))
        block.sync(mk("sp"))
        blk.__exit__(None, None, None)
        for g in reversed(sem_ctx):
            g.__exit__(None, None, None)
        for g in reversed(self._ctx):
            g.__exit__(None, None, None)
        return nc

    def eps_tile(self):
        return self._eps

    def matmul(self, out, lhsT, rhs, start=True, stop=True):
        self.op("pe", lambda e: e.matmul(out, lhsT, rhs, start=start, stop=stop),
                reads=[lhsT, rhs], writes=[out])

    def transpose(self, out, in_, ident):
        self.op("pe", lambda e: e.transpose(out, in_, ident), reads=[in_, ident], writes=[out])

    def act(self, out, in_, func, bias=None, scale=1.0, accum_out=None, eng="act", extra_reads=()):
        kw = {}
        rd = [in_] + list(extra_reads)
        wr = [out]
        if bias is not None:
            kw["bias"] = bias
            if not isinstance(bias, (int, float)):
                rd.append(bias)
        if not isinstance(scale, (int, float)):
            rd.append(scale)
        if accum_out is not None:
            kw["accum_out"] = accum_out
            wr.append(accum_out)
        self.op("act", lambda e: e.activation(out, in_, func, scale=scale, **kw), reads=rd, writes=wr)

    def tt(self, out, in0, in1, op, eng="dve"):
        self.op(eng, lambda e: e.tensor_tensor(out, in0, in1, op), reads=[in0, in1], writes=[out])

    def ts(self, out, in0, s1, op0, s2=None, op1=None, eng="dve", accum_out=None):
        rd = [in0]
        if not isinstance(s1, (int, float)):
            rd.append(s1)
        if s2 is not None and not isinstance(s2, (int, float)):
            rd.append(s2)
        wr = [out]
        kw = {}
        if accum_out is not None:
            kw["accum_out"] = accum_out
            wr.append(accum_out)
        if op1 is None:
            self.op(eng, lambda e: e.tensor_scalar(out, in0, s1, None, op0, **kw), reads=rd, writes=wr)
        else:
            self.op(eng, lambda e: e.tensor_scalar(out, in0, s1, s2, op0, op1, **kw), reads=rd, writes=wr)

    def stt(self, out, in0, scalar, in1, op0, op1, eng="dve", accum_out=None):
        rd = [in0, in1]
        if not isinstance(scalar, (int, float)):
            rd.append(scalar)
        wr = [out]
        kw = {}
        if accum_out is not None:
            kw["accum_out"] = accum_out
            wr.append(accum_out)
        self.op(eng, lambda e: e.scalar_tensor_tensor(out, in0, scalar, in1, op0, op1, **kw), reads=rd, writes=wr)

    def copy(self, out, in_, eng="dve"):
        if eng == "act":
            self.op("act", lambda e: e.copy(out, in_), reads=[in_], writes=[out])
        else:
            self.op(eng, lambda e: e.tensor_copy(out, in_), reads=[in_], writes=[out])

    def memset(self, ap, val, eng="dve"):
        self.op(eng, lambda e: e.memset(ap, val), reads=[], writes=[ap])

    def reduce(self, out, in_, op, axis=AX.X, eng="dve"):
        self.op(eng, lambda e: e.tensor_reduce(out, in_, axis, op), reads=[in_], writes=[out])

    def recip(self, out, in_):
        self.op("dve", lambda e: e.reciprocal(out, in_), reads=[in_], writes=[out])


D = 2048
NB = 4
SEQ = 8192
CTX = 256
DEPTH = 2
NCORE = 8
INC = 4928
EPS = 1e-6
_PROGS = {}


def _run(name, builder, in_maps):
    if name not in _PROGS:
        _PROGS[name] = builder
    nc = builder()
    res = run_bass_kernel_spmd(nc, in_maps, core_ids=list(range(NCORE)))
    return res.results


def build_mod():
    P = Prog()
    cT = P.dram("cT", [128, 16, 5], F32, "ExternalInput")
    wm = P.dram("wm", [128, 16, 2, 1536], F32, "ExternalInput")
    bm = P.dram("bm", [5, 2, 1536], F32, "ExternalInput")
    out = P.dram("mod", [5, 2, 1536], F32, "ExternalOutput")
    c_sb = P.sb("c_sb", [128, 16, 5], F32)
    s_sb = P.sb("s_sb", [128, 16, 5], F32)
    wbuf = P.sb("wbuf", [128, 2, 16, 512], F32)
    bsb = P.sb("bsb", [5, 2, 1536], F32)
    osb = P.sb("osb", [5, 2, 1536], F32)
    ps = P.ps("ps", [5, 2, 512], F32)
    P.dma(c_sb[:], cT[:])
    P.dma(bsb[:], bm[:])
    P.act(s_sb[:], c_sb[:], AF.Silu)
    i = 0
    for l in range(2):
        for nb in range(3):
            buf = i % 2
            P.dma(wbuf[:, buf], wm[:, :, l, nb * 512:(nb + 1) * 512])
            for k in range(16):
                P.matmul(ps[:, buf, :], s_sb[:, k, :], wbuf[:, buf, k, :], start=(k == 0), stop=(k == 15))
            P.tt(osb[:, l, nb * 512:(nb + 1) * 512], ps[:, buf, :], bsb[:, l, nb * 512:(nb + 1) * 512], ALU.add)
            i += 1
    P.dma(out[:], osb[:])
    return P.finish()


def run_mod(c, c_ctx, w_mod, b_mod):
    C5 = np.concatenate([c, c_ctx[None]], 0)
    cT = np.ascontiguousarray(C5.reshape(5, 16, 128).transpose(2, 1, 0))
    in_maps = []
    for i in range(NCORE):
        sl = slice(i * 1536, (i + 1) * 1536)
        wmi = np.ascontiguousarray(w_mod[:, :, sl].reshape(2, 16, 128, 1536).transpose(2, 1, 0, 3))
        bmi = np.ascontiguousarray(np.broadcast_to(b_mod[None, :, sl], (5, 2, 1536)))
        in_maps.append({"cT": cT, "wm": wmi, "bm": bmi})
    res = _run("mod", build_mod, in_maps)
    mod = np.concatenate([r["mod"] for r in res], axis=2)
    return mod


def bcast_mid(ap2, k):
    p, n = ap2.shape
    return ap2.unsqueeze(1).broadcast_to([p, k, n])


def fm_rstd(P, src, K, N, sq, ssp, ps, ones, rstd, dim):
    P.act(sq, src, AF.Square)
    if K > 1:
        P.reduce(ssp, sq.rearrange("p k t -> p t k"), ALU.add)
        red = ssp
    else:
        red = sq[:, 0, :]
    P.matmul(ps, ones, red)
    P.act(rstd, ps, AF.Sqrt, bias=P.eps_tile()[:rstd.shape[0], :], scale=1.0 / dim)
    P.recip(rstd, rstd)


NT1 = 4096 + 128


def build_proj():
    P = Prog()
    xT = P.dram("xT", [128, 16, NT1], F32, "ExternalInput")
    g1 = P.dram("g1", [128, 16], F32, "ExternalInput")
    sc = P.dram("sc", [128, 16, 2], F32, "ExternalInput")
    sh = P.dram("sh", [128, 16, 2], F32, "ExternalInput")
    w = P.dram("w", [128, 16, INC], F32, "ExternalInput")
    pT = P.dram("pT", [INC, NT1], F32, "ExternalOutput")

    hT = P.sb("hT", [128, 16, NT1], BF16)
    xs = P.sb("xs", [128, 2, 16, 128], F32)
    sq = P.sb("sq", [128, 16, 128], F32)
    ssp = P.sb("ssp", [128, 128], F32)
    rstd = P.sb("rstd", [128, 128], F32)
    ones = P.sb("ones", [128, 128], F32)
    g1s = P.sb("g1s", [128, 16], F32)
    scs = P.sb("scs", [128, 16, 2], F32)
    shs = P.sb("shs", [128, 16, 2], F32)
    G = P.sb("G", [128, 16, 2], F32)
    wb = P.sb("wb", [128, 2, 16, 128], BF16)
    ost = P.sb("ost", [128, 4, 512], F32)
    psn = P.ps("psn", [128, 128], F32)
    pso = P.ps("pso", [128, 4, 512], F32)

    P.memset(ones[:], 1.0)
    P.dma(g1s[:], g1[:])
    P.dma(scs[:], sc[:])
    P.dma(shs[:], sh[:])
    for j in range(2):
        P.stt(G[:, :, j], scs[:, :, j], 1.0, g1s[:], ALU.add, ALU.mult)
    for s in range(NT1 // 128):
        b = s % 2
        t0 = s * 128
        j = 0 if s < 32 else 1
        P.dma(xs[:, b], xT[:, :, t0:t0 + 128])
        fm_rstd(P, xs[:, b], 16, 128, sq[:], ssp[:], psn[:], ones[:], rstd[:], D)
        P.tt(sq[:], xs[:, b], bcast_mid(rstd[:], 16), ALU.mult)
        for k in range(16):
            P.act(hT[:, k, t0:t0 + 128], sq[:, k, :], AF.Identity, bias=shs[:, k, j:j + 1], scale=G[:, k, j:j + 1])
    groups = [(g * 512, 512) for g in range(8)] + [(4096, 128)]
    it = 0
    ncb = (INC + 127) // 128
    for cb in range(ncb):
        c0 = cb * 128
        ncols = min(128, INC - c0)
        wbuf = cb % 2
        P.dma(wb[:, wbuf, :, :ncols], w[:, :, c0:c0 + ncols], eng="pool")
        for (t0, n) in groups:
            pb = it % 4
            for k in range(16):
                P.matmul(pso[:ncols, pb, :n], wb[:, wbuf, k, :ncols], hT[:, k, t0:t0 + n], start=(k == 0), stop=(k == 15))
            if it % 2 == 0:
                P.copy(ost[:ncols, pb, :n], pso[:ncols, pb, :n], eng="act")
            else:
                P.copy(ost[:ncols, pb, :n], pso[:ncols, pb, :n], eng="dve")
            P.dma(pT[c0:c0 + ncols, t0:t0 + n], ost[:ncols, pb, :n])
            it += 1
    return P.finish()


def fm16(v):
    return np.ascontiguousarray(v.reshape(16, 128).T)


def to_fm(xtm):
    T = xtm.shape[0]
    return np.ascontiguousarray(xtm.reshape(T, 16, 128).transpose(2, 1, 0))


def run_proj(x_l, x_c, mod_l, norm1_g, w_in):
    wl = np.ascontiguousarray(w_in.reshape(16, 128, INC).transpose(1, 0, 2))
    g1 = fm16(norm1_g)
    in_maps = []
    for i in range(NCORE):
        b, h = i // 2, i % 2
        xt = np.concatenate([x_l[b, h * 4096:(h + 1) * 4096], x_c[b, h * 128:(h + 1) * 128]], 0)
        sc = np.stack([fm16(mod_l[b, D:2 * D]), fm16(mod_l[4, D:2 * D])], -1)
        sh = np.stack([fm16(mod_l[b, 0:D]), fm16(mod_l[4, 0:D])], -1)
        in_maps.append({"xT": to_fm(xt), "g1": g1, "sc": np.ascontiguousarray(sc), "sh": np.ascontiguousarray(sh), "w": wl})
    res = _run("proj", build_proj, in_maps)
    pT_l = np.empty((NB, INC, SEQ), np.float32)
    pT_c = np.empty((NB, INC, CTX), np.float32)
    for i in range(NCORE):
        b, h = i // 2, i % 2
        r = res[i]["pT"]
        pT_l[b, :, h * 4096:(h + 1) * 4096] = r[:, :4096]
        pT_c[b, :, h * 128:(h + 1) * 128] = r[:, 4096:]
    return pT_l, pT_c


MLA_SCALE = 192.0 ** -0.5


def add_groups(nt, width=512):
    g = []
    t = 0
    while t < nt:
        n = min(width, nt - t)
        g.append((t, n))
        t += n
    return g


def phase_mla(P, NT, cqT, ckvT, krT2, mixd, W, l, ident_d):
    NL = NT - CTX
    NKT = NT // 128
    with P.scope():
        ones = P.sb("ones", [128, 128], F32)
        P.memset(ones[:], 1.0)
        ckvn = P.sb("ckvn", [128, 2, NT], BF16)
        krr = P.sb("krr", [64, NT], BF16)
        gkv = P.sb("gkv", [128, 2], F32)
        gq = P.sb("gq", [128, 4], F32)
        P.dma(gkv[:], W["kvg"][l])
        P.dma(gq[:], W["qg"][l])
        cqn_d = P.dram(f"cqn_d{l}", [512, NT], BF16)
        st = P.sb("st", [128, 4, 512], F32)
        sq = P.sb("sq", [128, 4, 512], F32)
        ssp = P.sb("ssp", [128, 512], F32)
        rstd = P.sb("rstd", [128, 512], F32)
        psn = P.ps("psn", [128, 512], F32)
        kr2 = P.sb("kr2", [64, 2, 512], F32)
        cs = P.sb("cs", [64, 2, 512], F32)
        t1 = P.sb("t1", [64, 512], F32)
        t2 = P.sb("t2", [64, 512], F32)
        cqb = P.sb("cqb", [128, 4, 512], BF16)
        for (t0, n) in add_groups(NT):
            P.dma(st[:, :2, :n], ckvT.rearrange("(c p) t -> p c t", p=128)[:, :, t0:t0 + n])
            fm_rstd(P, st[:, :2, :n], 2, n, sq[:, :2, :n], ssp[:, :n], psn[:, :n], ones[:], rstd[:, :n], 256)
            P.tt(sq[:, :2, :n], st[:, :2, :n], bcast_mid(rstd[:, :n], 2), ALU.mult)
            for c in range(2):
                P.act(ckvn[:, c, t0:t0 + n], sq[:, c, :n], AF.Identity, scale=gkv[:, c:c + 1])
            P.dma(st[:, :, :n], cqT.rearrange("(c p) t -> p c t", p=128)[:, :, t0:t0 + n])
            fm_rstd(P, st[:, :, :n], 4, n, sq[:, :, :n], ssp[:, :n], psn[:, :n], ones[:], rstd[:, :n], 512)
            P.tt(sq[:, :, :n], st[:, :, :n], bcast_mid(rstd[:, :n], 4), ALU.mult)
            for c in range(4):
                P.act(cqb[:, c, :n], sq[:, c, :n], AF.Identity, scale=gq[:, c:c + 1])
            P.dma(cqn_d.rearrange("(c p) t -> p c t", p=128)[:, :, t0:t0 + n], cqb[:, :, :n])
            P.dma(kr2[:, :, :n], krT2.rearrange("(c p) t -> p c t", p=64)[:, :, t0:t0 + n])
            nc_ = max(0, min(n, CTX - t0))
            if nc_ > 0:
                P.copy(krr[:, t0:t0 + nc_], kr2[:, 0, :nc_])
            if nc_ < n:
                m = n - nc_
                l0 = t0 + nc_ - CTX
                P.dma(cs[:, 0, :m], W["cosT"][:, l0:l0 + m])
                P.dma(cs[:, 1, :m], W["sinT"][:, l0:l0 + m])
                P.tt(t1[:, :m], kr2[:, 0, nc_:n], cs[:, 0, :m], ALU.mult)
                P.tt(t2[:, :m], kr2[:, 1, nc_:n], cs[:, 1, :m], ALU.mult)
                P.tt(krr[:, t0 + nc_:t0 + n], t1[:, :m], t2[:, :m], ALU.add)
        wuk = P.sb("wuk", [128, 2, 128], BF16)
        wuv = P.sb("wuv", [128, 2, 128], BF16)
        wqn = P.sb("wqn", [128, 4, 128], BF16)
        wqr = P.sb("wqr", [128, 4, 2, 64], BF16)
        KhT = P.sb("KhT", [128, NT], BF16)
        Vh = P.sb("Vh", [128, NKT, 129], BF16)
        cqb2 = P.sb("cqb2", [128, 2, 4, 512], BF16)
        Qn2 = P.sb("Qn2", [128, 2, 512], BF16)
        Qr2 = P.sb("Qr2", [64, 2, 512], BF16)
        qr2b = P.sb("qr2b", [64, 2, 2, 512], F32)
        cs2 = P.sb("cs2", [64, 2, 2, 512], F32)
        PT = P.sb("PT", [128, 2, 512], BF16)
        rec = P.sb("rec", [128, 4], F32)
        osb = P.sb("osb", [128, 4, 128], F32)
        psk = P.ps("psk", [128, 512], F32)
        pss = P.ps("pss", [128, 2, 512], F32)
        acc = P.ps("acc", [128, 4, 512], F32)
        P.memset(Vh[:, :, 128:129], 1.0)
        for h in range(8):
            P.dma(wuk[:], W["wuk"][l, :, :, h, :], eng="pool")
            P.dma(wuv[:], W["wuv"][l, :, :, h, :], eng="pool")
            P.dma(wqn[:], W["wqn"][l, :, :, h, :], eng="pool")
            P.dma(wqr[:], W["wqr"][l, :, :, h, :, :], eng="pool")
            pk2 = [psk, psn]
            pi = 0
            for (t0, n) in add_groups(NT):
                pp_ = pk2[pi % 2]
                pi += 1
                for c in range(2):
                    P.matmul(pp_[:, :n], wuk[:, c, :], ckvn[:, c, t0:t0 + n], start=(c == 0), stop=(c == 1))
                P.copy(KhT[:, t0:t0 + n], pp_[:, :n], eng="act")
            for kt in range(NKT):
                pp_ = pk2[pi % 2]
                pi += 1
                for c in range(2):
                    P.matmul(pp_[:, :128], ckvn[:, c, kt * 128:(kt + 1) * 128], wuv[:, c, :], start=(c == 0), stop=(c == 1))
                P.copy(Vh[:, kt, :128], pp_[:, :128], eng="dve")
            qgroups = [(0, CTX, True)] + [(CTX + a, b, False) for (a, b) in add_groups(NL)]

            def prologue(gi):
                t0, n, isctx = qgroups[gi]
                pb = gi % 2
                P.dma(cqb2[:, pb, :, :n], cqn_d.rearrange("(c p) t -> p c t", p=128)[:, :, t0:t0 + n])
                for c in range(4):
                    P.matmul(psk[:, :n], wqn[:, c, :], cqb2[:, pb, c, :n], start=(c == 0), stop=(c == 3))
                P.copy(Qn2[:, pb, :n], psk[:, :n], eng="act")
                for j in range(2):
                    pj = psn if j == 0 else psk
                    for c in range(4):
                        P.matmul(pj[:64, :n], wqr[:, c, j, :], cqb2[:, pb, c, :n], start=(c == 0), stop=(c == 3))
                    P.copy(qr2b[:, pb, j, :n], pj[:64, :n], eng="dve")
                    if isctx:
                        break
                if isctx:
                    P.copy(Qr2[:, pb, :n], qr2b[:, pb, 0, :n])
                else:
                    l0 = t0 - CTX
                    P.dma(cs2[:, pb, 0, :n], W["cosT"][:, l0:l0 + n])
                    P.dma(cs2[:, pb, 1, :n], W["sinT"][:, l0:l0 + n])
                    P.tt(t1[:, :n], qr2b[:, pb, 0, :n], cs2[:, pb, 0, :n], ALU.mult)
                    P.tt(t2[:, :n], qr2b[:, pb, 1, :n], cs2[:, pb, 1, :n], ALU.mult)
                    P.tt(Qr2[:, pb, :n], t1[:, :n], t2[:, :n], ALU.add)

            prologue(0)
            for gi, (t0, n, isctx) in enumerate(qgroups):
                if gi + 1 < len(qgroups):
                    prologue(gi + 1)
                pb = gi % 2
                kts = range(CTX // 128) if isctx else range(NKT)
                nq = n // 128
                last = len(kts) - 1
                for i, kt in enumerate(kts):
                    b = i % 2
                    P.matmul(pss[:, b, :n], KhT[:, kt * 128:(kt + 1) * 128], Qn2[:, pb, :n], start=True, stop=False)
                    P.matmul(pss[:, b, :n], krr[:, kt * 128:(kt + 1) * 128], Qr2[:, pb, :n], start=False, stop=True)
                    P.act(PT[:, b, :n], pss[:, b, :n], AF.Exp, scale=MLA_SCALE)
                    for qb in range(nq):
                        P.matmul(acc[:, qb, :129], PT[:, b, qb * 128:(qb + 1) * 128], Vh[:, kt, :], start=(i == 0), stop=(i == last))
                for qb in range(nq):
                    P.recip(rec[:, qb:qb + 1], acc[:, qb, 128:129])
                    P.ts(osb[:, qb, :], acc[:, qb, :128], rec[:, qb:qb + 1], ALU.mult)
                P.dma(mixd[t0:t0 + n, h * 128:(h + 1) * 128].rearrange("(q p) d -> p q d", p=128), osb[:, :nq, :])


def rope_tables_T(n):
    t = np.arange(n)
    inv = 10000.0 ** (-np.arange(16, dtype=np.float32) / 16)
    ang_r = (t // 64).astype(np.float32)[:, None] * inv
    ang_c = (t % 64).astype(np.float32)[:, None] * inv
    cosT = np.concatenate([np.cos(ang_r), np.cos(ang_r), np.cos(ang_c), np.cos(ang_c)], 1).T
    sinT = np.concatenate([-np.sin(ang_r), np.sin(ang_r), -np.sin(ang_c), np.sin(ang_c)], 1).T
    return np.ascontiguousarray(cosT.astype(np.float32)), np.ascontiguousarray(sinT.astype(np.float32))


SW64 = np.array([i + 16 if (i % 32) < 16 else i - 16 for i in range(64)])


def mla_weights(mla_q_norm_g, mla_kv_norm_g, mla_w_uq, mla_w_uk, mla_w_uv):
    L = mla_w_uq.shape[0]
    W = {}
    W["kvg"] = np.ascontiguousarray(mla_kv_norm_g.reshape(L, 2, 128).transpose(0, 2, 1))
    W["qg"] = np.ascontiguousarray(mla_q_norm_g.reshape(L, 4, 128).transpose(0, 2, 1))
    W["wuk"] = np.ascontiguousarray(mla_w_uk.reshape(L, 2, 128, 8, 128).transpose(0, 2, 1, 3, 4))
    W["wuv"] = np.ascontiguousarray(mla_w_uv.reshape(L, 2, 128, 8, 128).transpose(0, 2, 1, 3, 4))
    uq = mla_w_uq.reshape(L, 4, 128, 8, 192).transpose(0, 2, 1, 3, 4)
    W["wqn"] = np.ascontiguousarray(uq[..., :128])
    r = uq[..., 128:]
    W["wqr"] = np.ascontiguousarray(np.stack([r, r[..., SW64]], axis=-2))
    return W


NAT_SCALE = 128.0 ** -0.5
NEG = -30000.0


def nat_case(r, NR):
    if r == 0:
        return 0, 0, 9
    if r == 2:
        return 1, 0, 9
    if r == NR - 4:
        return 3, NR - 8, 8
    if r == NR - 2:
        return 4, NR - 8, 8
    return 2, r - 4, 9


def nat_bias_tables(rpb):
    H = rpb.shape[0]
    out = np.full((H, 5, 128, 576), NEG, np.float32)
    cases = [((7, 0), (6, 0)), ((5, 0), (4, 0)), ((3, 0), (2, 1)), ((3, 0), (2, 0)), ((1, 0), (0, 0))]
    qc = np.arange(64)
    c0 = np.clip(qc - 8, 0, 48)
    for ci, cs in enumerate(cases):
        for qr in range(2):
            off, i0 = cs[qr]
            for i in range(i0, i0 + 8):
                dr = i + off
                for q in range(64):
                    kc = np.arange(c0[q], c0[q] + 16)
                    out[:, ci, qr * 64 + q, i * 64 + kc] = rpb[:, dr, kc - q + 15]
    return out


def phase_nat(P, NT, nqT, nkT, nv, mixd, natb, l, identb_d):
    NL = NT - CTX
    NR = NL // 64
    with P.scope():
        ident = P.sb("identb", [128, 128], BF16)
        P.dma(ident[:], identb_d[:], eng="pool")
        KT = P.sb("KT", [128, NT], BF16)
        QT = P.sb("QT", [128, NT], BF16)
        Vrow = P.sb("Vrow", [64, NR, 128], BF16)
        Vctx = P.sb("Vctx", [128, 2, 128], BF16)
        bias = P.sb("bias", [128, 5, 576], BF16)
        Pb = P.sb("Pb", [128, 2, 832], BF16)
        PT1 = P.sb("PT1", [64, 2, 8, 128], BF16)
        PT2 = P.sb("PT2", [128, 2, 3, 128], BF16)
        mx = P.sb("mx", [128, 2], F32)
        rs = P.sb("rs", [128, 2], F32)
        osb = P.sb("osb", [128, 2, 128], F32)
        S = P.ps("S", [128, 2, 1024], F32)
        pt1 = P.ps("pt1", [64, 8, 128], BF16)
        pt2 = P.ps("pt2", [128, 3, 128], BF16)
        O = P.ps("O", [128, 2, 128], F32)
        for h in range(4):
            hs = slice(h * 128, (h + 1) * 128)
            P.dma(KT[:], nkT[hs, :])
            P.dma(QT[:], nqT[hs, :])
            P.act(QT[:], QT[:], AF.Copy, scale=NAT_SCALE)
            P.dma(Vrow[:], nv[CTX:, hs].rearrange("(r c) d -> c r d", c=64))
            P.dma(Vctx[:], nv[0:CTX, hs].rearrange("(j p) d -> p j d", p=128))
            P.dma(bias[:], natb[l, h].rearrange("c p k -> p c k"), eng="pool")
            units = [("ctx", qt) for qt in range(2)] + [("lat", pr) for pr in range(NR // 2)]
            for ui, (kind, idx) in enumerate(units):
                b = ui % 2
                if kind == "ctx":
                    q0 = idx * 128
                    nrows = 0
                    woff = 0
                    P.matmul(S[:, b, 0:256], QT[:, q0:q0 + 128], KT[:, 0:CTX], start=True, stop=True)
                else:
                    r = idx * 2
                    ci, R0, nrows = nat_case(r, NR)
                    q0 = CTX + r * 64
                    k0 = CTX + R0 * 64
                    woff = nrows * 64
                    P.matmul(S[:, b, 0:512], QT[:, q0:q0 + 128], KT[:, k0:k0 + 512], start=True, stop=False)
                    P.matmul(S[:, b, 0:512], ident[:], bias[:, ci, 0:512], start=False, stop=True)
                    if nrows == 9:
                        P.matmul(S[:, b, 512:576], QT[:, q0:q0 + 128], KT[:, k0 + 512:k0 + 576], start=True, stop=False)
                        P.matmul(S[:, b, 512:576], ident[:], bias[:, ci, 512:576], start=False, stop=True)
                    P.matmul(S[:, b, woff:woff + 256], QT[:, q0:q0 + 128], KT[:, 0:CTX], start=True, stop=True)
                tot = woff + 256
                P.reduce(mx[:, b:b + 1], S[:, b, :tot], ALU.max)
                P.ts(mx[:, b:b + 1], mx[:, b:b + 1], -1.0, ALU.mult)
                P.act(Pb[:, b, :tot], S[:, b, :tot], AF.Exp, bias=mx[:, b:b + 1], scale=1.0, accum_out=rs[:, b:b + 1])
                for i in range(min(nrows, 8)):
                    P.transpose(pt1[:, i, :], Pb[:, b, i * 64:(i + 1) * 64], ident[:])
                if nrows == 9:
                    P.transpose(pt2[:64, 0, :], Pb[:, b, 512:576], ident[:])
                for j in range(2):
                    P.transpose(pt2[:, 1 + j, :], Pb[:, b, woff + j * 128:woff + (j + 1) * 128], ident[:])
                if nrows:
                    P.copy(PT1[:, b], pt1[:], eng="act")
                P.copy(PT2[:, b], pt2[:], eng="dve")
                mms = []
                for i in range(min(nrows, 8)):
                    mms.append((PT1[:, b, i, :], Vrow[:, R0 + i, :]))
                if nrows == 9:
                    mms.append((PT2[:64, b, 0, :], Vrow[:, R0 + 8, :]))
                for j in range(2):
                    mms.append((PT2[:, b, 1 + j, :], Vctx[:, j, :]))
                for mi, (lt, rh) in enumerate(mms):
                    P.matmul(O[:, b, :], lt, rh, start=(mi == 0), stop=(mi == len(mms) - 1))
                P.recip(rs[:, b:b + 1], rs[:, b:b + 1])
                P.ts(osb[:, b, :], O[:, b, :], rs[:, b:b + 1], ALU.mult)
                P.dma(mixd[q0:q0 + 128, 1024 + h * 128:1024 + (h + 1) * 128], osb[:, b, :])


def ret_consts():
    s = np.arange(128)[:, None].astype(np.float32)
    c = np.arange(128)[None, :].astype(np.float32)
    E = np.stack([np.maximum(c - s, 0), np.maximum(s - c, 0)]).astype(np.float32)
    M = np.stack([(c >= s), (s >= c)]).astype(np.float32)
    p = np.arange(128).astype(np.float32)
    wkexp = np.stack([127 - p, p], 1).astype(np.float32)
    wq = np.stack([np.arange(128) + 1.0, 128.0 - np.arange(128)]).astype(np.float32)
    wqexp = np.ascontiguousarray(np.broadcast_to(wq[None], (128, 2, 128))).astype(np.float32)
    return {"retE": E, "retM": M, "retwk": wkexp, "retwq": wqexp}


def phase_ret(P, NT, rqT, rkT, rk, rv, gfb, mixd, C, l):
    NKT = NT // 128
    with P.scope():
        dec = P.sb("dec", [128, 8], F32)
        lg = P.sb("lg", [128, 8], F32)
        gch = P.sb("gch", [128, 8], F32)
        E = P.sb("E", [128, 2, 128], F32)
        M = P.sb("M", [128, 2, 128], F32)
        wke = P.sb("wke", [128, 2], F32)
        wqe = P.sb("wqe", [128, 2, 128], F32)
        P.dma(dec[:], C["dec"][l])
        P.dma(E[:], C["retE"].rearrange("d s c -> s d c"))
        P.dma(M[:], C["retM"].rearrange("d s c -> s d c"))
        P.dma(wke[:], C["retwk"][:])
        P.dma(wqe[:], C["retwq"][:])
        P.act(lg[:], dec[:], AF.Sigmoid)
        P.act(lg[:], lg[:], AF.Ln)
        P.act(gch[:], lg[:], AF.Exp, scale=128.0)
        dmT = P.sb("dmT", [128, 128], F32)
        wk = P.sb("wk", [128, 1], F32)
        wqr = P.sb("wqr", [128, 128], F32)
        KT = P.sb("KT", [128, NT], BF16)
        QT = P.sb("QT", [128, NT], BF16)
        Ktm = P.sb("Ktm", [128, NKT, 128], BF16)
        Vtm = P.sb("Vtm", [128, NKT, 128], BF16)
        rbuf = P.sb("rbuf", [128, NKT, 128], F32)
        Sf = P.sb("Sf", [128, 128], F32)
        Sb = P.sb("Sb", [128, 128], BF16)
        kw = P.sb("kw", [128, 2, 128], BF16)
        sTm = P.sb("sTm", [128, 2, 128], BF16)
        Qw = P.sb("Qw", [128, 2, 128], BF16)
        gt = P.sb("gt", [128, 2, 128], F32)
        ocp = P.sb("ocp", [128, 2, 128], F32)
        junk = P.sb("junk", [128, 128], F32)
        st = P.sb("st", [128, 2, 8], F32)
        pkv = P.ps("pkv", [128, 2, 128], F32)
        psT = P.ps("psT", [128, 2, 128], F32)
        po = P.ps("po", [128, 2, 128], F32)
        fwd = list(range(NKT))
        bwd = [1, 0] + list(range(NKT - 1, 1, -1))
        for h in range(4):
            hs = slice(h * 128, (h + 1) * 128)
            P.dma(KT[:], rkT[hs, :])
            P.dma(QT[:], rqT[hs, :])
            P.dma(Ktm[:], rk[:, hs].rearrange("(j p) d -> p j d", p=128))
            P.dma(Vtm[:], rv[:, hs].rearrange("(j p) d -> p j d", p=128))
            for d in range(2):
                hd = d * 4 + h
                P.act(dmT[:], E[:, d, :], AF.Exp, scale=lg[:, hd:hd + 1])
                P.tt(dmT[:], dmT[:], M[:, d, :], ALU.mult)
                P.act(wk[:], wke[:, d:d + 1], AF.Exp, scale=lg[:, hd:hd + 1])
                P.act(wqr[:], wqe[:, d, :], AF.Exp, scale=lg[:, hd:hd + 1])
                P.memset(Sf[:], 0.0)
                P.memset(Sb[:], 0.0)
                for ci, tt in enumerate(fwd if d == 0 else bwd):
                    b = ci % 2
                    ts_ = slice(tt * 128, (tt + 1) * 128)
                    P.ts(kw[:, b, :], Ktm[:, tt, :], wk[:, 0:1], ALU.mult)
                    P.matmul(pkv[:, b, :], kw[:, b, :], Vtm[:, tt, :])
                    P.matmul(psT[:, b, :], KT[:, ts_], QT[:, ts_])
                    P.tt(sTm[:, b, :], psT[:, b, :], dmT[:], ALU.mult)
                    P.tt(Qw[:, b, :], QT[:, ts_], wqr[:], ALU.mult, eng="pool")
                    P.matmul(po[:, b, :], sTm[:, b, :], Vtm[:, tt, :], start=True, stop=False)
                    P.matmul(po[:, b, :], Qw[:, b, :], Sb[:], start=False, stop=True)
                    P.stt(Sf[:], Sf[:], gch[:, hd:hd + 1], pkv[:, b, :], ALU.mult, ALU.add)
                    P.copy(Sb[:], Sf[:], eng="act")
                    P.dma(gt[:, b, :], gfb[ts_, d * 512 + h * 128:d * 512 + (h + 1) * 128])
                    s = st[:, b, :]
                    P.act(ocp[:, b, :], po[:, b, :], AF.Identity, accum_out=s[:, 0:1])
                    P.act(junk[:], po[:, b, :], AF.Square, accum_out=s[:, 1:2])
                    P.ts(s[:, 2:3], s[:, 0:1], 1.0 / 128, ALU.mult)
                    P.tt(s[:, 3:4], s[:, 2:3], s[:, 2:3], ALU.mult)
                    P.stt(s[:, 4:5], s[:, 1:2], 1.0 / 128, s[:, 3:4], ALU.mult, ALU.subtract)
                    P.act(s[:, 5:6], s[:, 4:5], AF.Sqrt, bias=P.eps_tile()[:, :], scale=1.0)
                    P.recip(s[:, 5:6], s[:, 5:6])
                    P.ts(ocp[:, b, :], ocp[:, b, :], s[:, 2:3], ALU.subtract, s[:, 5:6], ALU.mult)
                    P.act(gt[:, b, :], gt[:, b, :], AF.Silu)
                    if d == 0:
                        P.tt(rbuf[:, tt, :], gt[:, b, :], ocp[:, b, :], ALU.mult)
                    else:
                        P.tt(ocp[:, b, :], gt[:, b, :], ocp[:, b, :], ALU.mult)
                        P.tt(ocp[:, b, :], ocp[:, b, :], rbuf[:, tt, :], ALU.add)
                        P.dma(mixd[ts_, 1536 + h * 128:1536 + (h + 1) * 128], ocp[:, b, :])


RET_KS = 128.0 ** -0.5
FMW = 2944
TMW = 2560


def bcrow(row):
    n = row.shape[-1]
    return bass.AP(row.tensor, row.offset, [[0, 128], [1, n]])


def phase_proj2(P, NT, x_s, S, A, l, parts=(1, 1, 1)):
    NKT = NT // 128
    with P.scope():
        identb = P.sb("identb", [128, 128], BF16)
        P.dma(identb[:], A["ident"][:], eng="pool")
        mf = P.sb("mf", [128, 16, 8], F32)
        P.dma(mf[:], A["modfm"][l])
        n1 = P.sb("n1", [128, 16], F32)
        P.dma(n1[:], A["n1g"][l])
        G = P.sb("G", [128, 16, 2], F32)
        for j in range(2):
            P.stt(G[:, :, j], mf[:, :, 1 + 2 * j], 1.0, n1[:], ALU.add, ALU.mult)
        hT = P.sb("hT", [128, 16, 18 * 128], BF16)
        xt = P.sb("xt", [128, 2, D], F32)
        xn = P.sb("xn", [128, 2, D], BF16)
        ss = P.sb("ss", [128, 2], F32)
        wf = P.sb("wf", [128, 2, 16, 128], BF16)
        wt = P.sb("wt", [128, 2, 16, 256], BF16)
        oF = P.sb("oF", [128, 2, 512], F32)
        oB = P.sb("oB", [128, 2, 512], BF16)
        pt = P.ps("pt", [128, 16, 128], BF16)
        pso = P.ps("pso", [128, 2, 512], F32)
        tmpT = P.sb("tmpT", [128, 16, 128], F32)
        eps = P.eps_tile()
        chunks = []
        t = 0
        while t < NKT:
            n = min(18 if t == 0 else 16, NKT - t)
            chunks.append((t, n))
            t += n
        it = 0
        wi = 0
        for (tl0, ntl) in chunks:
            for i in range(ntl):
                tl = tl0 + i
                b = tl % 2
                j = 1 if tl < 2 else 0
                P.dma(xt[:, b], x_s[tl * 128:(tl + 1) * 128, :])
                P.act(xn[:, b], xt[:, b], AF.Square, accum_out=ss[:, b:b + 1])
                P.act(ss[:, b:b + 1], ss[:, b:b + 1], AF.Sqrt, bias=eps[:, :], scale=1.0 / D)
                P.recip(ss[:, b:b + 1], ss[:, b:b + 1])
                P.ts(xn[:, b], xt[:, b], ss[:, b:b + 1], ALU.mult)
                for k in range(16):
                    P.transpose(pt[:, k, :], xn[:, b, k * 128:(k + 1) * 128], identb[:])
                P.copy(tmpT[:], pt[:], eng="act")
                P.tt(tmpT[:], tmpT[:], G[:, :, j].unsqueeze(2).broadcast_to([128, 16, 128]), ALU.mult, eng="pool")
                P.tt(hT[:, :, i * 128:(i + 1) * 128], tmpT[:], mf[:, :, 2 * j].unsqueeze(2).broadcast_to([128, 16, 128]), ALU.add)
            groups = []
            g0 = 0
            if tl0 == 0:
                groups.append((0, 2))
                g0 = 2
            while g0 < ntl:
                gn = min(4, ntl - g0)
                groups.append((g0, gn))
                g0 += gn
            for cb in range(FMW // 128 if parts[1] else 0):
                wb = wi % 2
                wi += 1
                P.dma(wf[:, wb].rearrange("p k n -> p (k n)"), A["w_fm"][l, cb], eng="pool")
                if cb < 4:
                    dest, r0, isbf, scl = S["cqT"], cb * 128, False, 1.0
                elif cb < 6:
                    dest, r0, isbf, scl = S["ckvT"], (cb - 4) * 128, False, 1.0
                elif cb == 6:
                    dest, r0, isbf, scl = S["krT2"], 0, False, 1.0
                elif cb < 11:
                    dest, r0, isbf, scl = S["nqT"], (cb - 7) * 128, True, 1.0
                elif cb < 15:
                    dest, r0, isbf, scl = S["nkT"], (cb - 11) * 128, True, 1.0
                elif cb < 19:
                    dest, r0, isbf, scl = S["rqT"], (cb - 15) * 128, True, 1.0
                else:
                    dest, r0, isbf, scl = S["rkT"], (cb - 19) * 128, True, RET_KS
                for (g0, gn) in groups:
                    n = gn * 128
                    c0 = g0 * 128
                    tok0 = (tl0 + g0) * 128
                    pb = it % 2
                    for k in range(16):
                        P.matmul(pso[:, pb, :n], wf[:, wb, k, :], hT[:, k, c0:c0 + n], start=(k == 0), stop=(k == 15))
                    stage = oB if isbf else oF
                    if scl != 1.0:
                        P.act(stage[:, pb, :n], pso[:, pb, :n], AF.Copy, scale=scl)
                    else:
                        P.copy(stage[:, pb, :n], pso[:, pb, :n], eng=("act" if it % 2 == 0 else "dve"))
                    P.dma(dest[r0:r0 + 128, tok0:tok0 + n], stage[:, pb, :n])
                    it += 1
            for tb in range(TMW // 256 if parts[2] else 0):
                wb = wi % 2
                wi += 1
                P.dma(wt[:, wb].rearrange("p k n -> p (k n)"), A["w_tm"][l, tb], eng="pool")
                if tb < 2:
                    dest, c0, isbf, scl = S["nv"], tb * 256, True, 1.0
                elif tb < 4:
                    dest, c0, isbf, scl = S["rk"], (tb - 2) * 256, True, RET_KS
                elif tb < 6:
                    dest, c0, isbf, scl = S["rv"], (tb - 4) * 256, True, 1.0
                else:
                    dest, c0, isbf, scl = S["gfb"], (tb - 6) * 256, False, 1.0
                for i in range(ntl):
                    tl = tl0 + i
                    pb = it % 2
                    for k in range(16):
                        P.matmul(pso[:, pb, :256], hT[:, k, i * 128:(i + 1) * 128], wt[:, wb, k, :], start=(k == 0), stop=(k == 15))
                    stage = oB if isbf else oF
                    if scl != 1.0:
                        P.act(stage[:, pb, :256], pso[:, pb, :256], AF.Copy, scale=scl)
                    else:
                        P.copy(stage[:, pb, :256], pso[:, pb, :256], eng=("act" if it % 2 == 0 else "dve"))
                    P.dma(dest[tl * 128:(tl + 1) * 128, c0:c0 + 256], stage[:, pb, :256])
                    it += 1


def phase_post(P, NT, x_s, S, A, l):
    NKT = NT // 128
    with P.scope():
        identb = P.sb("identb", [128, 128], BF16)
        P.dma(identb[:], A["ident"][:], eng="pool")
        identf = P.sb("identf", [128, 128], F32)
        P.dma(identf[:], A["ident"][:])
        wo = P.sb("wo", [128, 16, D], BF16)
        for q in range(4):
            P.dma(wo[:, q * 4:(q + 1) * 4].rearrange("p k f -> p (k f)"), A["w_out"][l, :, q * 4:(q + 1) * 4, :].rearrange("p k f -> p (k f)"), eng="pool")
        wr = P.sb("wr", [128, 16, 16], F32)
        P.dma(wr[:], A["w_r"][l])
        ong = P.sb("ong", [128, 16], F32)
        P.dma(ong[:], A["ong"][l])
        g1r = P.sb("g1r", [128, 2, D], F32)
        G2r = P.sb("G2r", [128, 2, D], F32)
        S2r = P.sb("S2r", [128, 2, D], F32)
        n2r = P.sb("n2r", [128, D], F32)
        P.dma(n2r[:], bcrow(A["n2g_row"][l]))
        for j in range(2):
            P.dma(g1r[:, j], bcrow(A["modrow"][l, j:j + 1, :]))
            P.dma(G2r[:, j], bcrow(A["modrow"][l, 4 + 2 * j:5 + 2 * j, :]))
            P.dma(S2r[:, j], bcrow(A["modrow"][l, 5 + 2 * j:6 + 2 * j, :]))
            P.stt(G2r[:, j], G2r[:, j], 1.0, n2r[:], ALU.add, ALU.mult)
        mx = P.sb("mx", [128, D], F32)
        xt = P.sb("xt", [128, D], F32)
        junk = P.sb("junk", [128, D], BF16)
        yn = P.sb("yn", [128, D], BF16)
        ynT = P.sb("ynT", [128, 16, 128], BF16)
        xnew = P.sb("xnew", [128, D], F32)
        h2f = P.sb("h2f", [128, D], F32)
        h2b = P.sb("h2b", [128, D], BF16)
        h2T = P.sb("h2T", [128, 16, 128], F32)
        s3 = P.sb("s3", [128, 8], F32)
        ex = P.sb("ex", [128, 16], F32)
        af = P.sb("af", [128, 16], F32)
        psT = P.ps("psT", [128, 16, 128], BF16)
        tmpT = P.sb("tmpT", [128, 16, 128], F32)
        psA = P.ps("psA", [128, 4, 512], F32)
        psr = P.ps("psr", [128, 16], F32)
        eps = P.eps_tile()
        psA_flat = psA[:].rearrange("p a b -> p (a b)")
        psA_t = psA[:].rearrange("p a (c d) -> p (a c) d", d=128)
        grp = [(0, 1024), (1024, 1536), (1536, 2048)]
        for tl in range(NKT):
            rows = slice(tl * 128, (tl + 1) * 128)
            j = 1 if tl < 2 else 0
            P.dma(mx[:], S["mix"][rows, :])
            P.dma(xt[:], x_s[rows, :])
            for i, (a0, a1) in enumerate(grp):
                P.act(junk[:, a0:a1], mx[:, a0:a1], AF.Square, accum_out=s3[:, i:i + 1])
                P.act(s3[:, i:i + 1], s3[:, i:i + 1], AF.Sqrt, bias=eps[:, :], scale=1.0 / (a1 - a0))
            P.recip(s3[:, 0:3], s3[:, 0:3])
            for i, (a0, a1) in enumerate(grp):
                P.ts(yn[:, a0:a1], mx[:, a0:a1], s3[:, i:i + 1], ALU.mult)
            for k in range(16):
                P.transpose(psT[:, k, :], yn[:, k * 128:(k + 1) * 128], identb[:])
            P.copy(tmpT[:], psT[:], eng="act")
            P.tt(ynT[:], tmpT[:], ong[:].unsqueeze(2).broadcast_to([128, 16, 128]), ALU.mult, eng="pool")
            for nb in range(4):
                for k in range(16):
                    P.matmul(psA[:, nb, :], ynT[:, k, :], wo[:, k, nb * 512:(nb + 1) * 512], start=(k == 0), stop=(k == 15))
            P.tt(xnew[:], psA_flat, g1r[:, j], ALU.mult)
            P.tt(xnew[:], xnew[:], xt[:], ALU.add)
            P.dma(x_s[rows, :], xnew[:])
            P.act(junk[:], xnew[:], AF.Square, accum_out=s3[:, 3:4])
            P.act(s3[:, 3:4], s3[:, 3:4], AF.Sqrt, bias=eps[:, :], scale=1.0 / D)
            P.recip(s3[:, 3:4], s3[:, 3:4])
            P.ts(h2f[:], xnew[:], s3[:, 3:4], ALU.mult)
            P.tt(h2f[:], h2f[:], G2r[:, j], ALU.mult)
            P.tt(h2f[:], h2f[:], S2r[:, j], ALU.add)
            P.copy(h2b[:], h2f[:], eng="pool")
            P.dma(S["h2"][rows, :], h2b[:])
            for k in range(16):
                P.transpose(psA_t[:, k, :], h2f[:, k * 128:(k + 1) * 128], identf[:])
            P.copy(h2T[:], psA_t, eng="act")
            for k in range(16):
                P.matmul(psr[:], h2T[:, k, :], wr[:, k, :], start=(k == 0), stop=(k == 15))
            P.reduce(s3[:, 4:5], psr[:], ALU.max)
            P.ts(s3[:, 4:5], s3[:, 4:5], -1.0, ALU.mult)
            P.act(ex[:], psr[:], AF.Exp, bias=s3[:, 4:5], scale=1.0, accum_out=s3[:, 5:6])
            P.recip(s3[:, 5:6], s3[:, 5:6])
            P.ts(af[:], ex[:], s3[:, 5:6], ALU.mult)
            P.dma(S["aff"][rows, :], af[:])


def phase_moe(P, NT, x_s, S, A, l, FF, last, out_d):
    NKT = NT // 128
    NL = NT - CTX
    NJL = NKT - 2
    KL = NL // 8
    KC = CTX // 8
    LCAP = KL + 128
    NSLOT = LCAP + 128
    ZROW = NSLOT
    NFC = FF // 128
    BIG = 1.0e6
    with P.scope():
        a2 = P.sb("a2", [128, NKT, 16], F32)
        P.dma(a2[:], S["aff"].rearrange("(j p) e -> p j e", p=128))
        ones = P.sb("ones", [128, 128], F32)
        P.memset(ones[:], 1.0)
        tri = P.sb("tri", [128, 128], F32)
        P.dma(tri[:], A["tri"][:])
        m = P.sb("m", [128, NKT, 16], F32)
        pc = P.ps("pc", [128, 16], F32)
        for (j0, nj, kcap) in ((2, NJL, KL), (0, 2, KC)):
            a = P.sb("a", [128, 16, nj], F32)
            P.copy(a[:], a2[:, j0:j0 + nj, :].rearrange("p j e -> p e j"))
            lo = P.sb("lo", [128, 16], F32)
            hi = P.sb("hi", [128, 16], F32)
            mid = P.sb("mid", [128, 16], F32)
            ge = P.sb("ge", [128, 16], F32)
            dlt = P.sb("dlt", [128, 16], F32)
            msk = P.sb("msk", [128, 16, nj], F32)
            cntp = P.sb("cntp", [128, 16], F32)
            P.memset(lo[:], 0.0)
            P.memset(hi[:], 1.0)
            for it in range(34):
                P.tt(mid[:], lo[:], hi[:], ALU.add)
                P.ts(mid[:], mid[:], 0.5, ALU.mult)
                P.tt(msk[:], a[:], mid[:].unsqueeze(2).broadcast_to([128, 16, nj]), ALU.is_ge)
                P.reduce(cntp[:], msk[:], ALU.add)
                P.matmul(pc[:], ones[:], cntp[:])
                P.ts(ge[:], pc[:], float(kcap), ALU.is_ge)
                P.tt(dlt[:], mid[:], lo[:], ALU.subtract)
                P.tt(dlt[:], dlt[:], ge[:], ALU.mult)
                P.tt(lo[:], lo[:], dlt[:], ALU.add)
                P.tt(dlt[:], hi[:], mid[:], ALU.subtract)
                P.tt(dlt[:], dlt[:], ge[:], ALU.mult)
                P.tt(hi[:], mid[:], dlt[:], ALU.add)
            P.tt(m[:, j0:j0 + nj, :], a2[:, j0:j0 + nj, :], lo[:].unsqueeze(1).broadcast_to([128, nj, 16]), ALU.is_ge)
        gate = P.sb("gate", [128, NKT, 16], F32)
        P.tt(gate[:], a2[:], m[:], ALU.mult)
        P.dma(S["gate"][:], gate[:])
        pre = P.sb("pre", [128, NKT, 16], F32)
        tot = P.sb("tot", [128, NKT, 16], F32)
        pp = P.ps("pp", [128, 512], F32)
        ptt = P.ps("ptt", [128, 512], F32)
        m2d = m[:].rearrange("p j e -> p (j e)")
        pre2d = pre[:].rearrange("p j e -> p (j e)")
        tot2d = tot[:].rearrange("p j e -> p (j e)")
        for c0 in range(0, NKT * 16, 512):
            n = min(512, NKT * 16 - c0)
            P.matmul(pp[:, :n], tri[:], m2d[:, c0:c0 + n])
            P.matmul(ptt[:, :n], ones[:], m2d[:, c0:c0 + n])
            P.copy(pre2d[:, c0:c0 + n], pp[:, :n], eng="act")
            P.copy(tot2d[:, c0:c0 + n], ptt[:, :n], eng="dve")
        offs = P.sb("offs", [128, NKT, 16], F32)
        P.memset(offs[:, 0, :], float(LCAP))
        P.tt(offs[:, 1, :], offs[:, 0, :], tot[:, 0, :], ALU.add)
        P.memset(offs[:, 2, :], 0.0)
        for j in range(3, NKT):
            P.tt(offs[:, j, :], offs[:, j - 1, :], tot[:, j - 1, :], ALU.add)
        pos = P.sb("pos", [128, NKT, 16], F32)
        P.tt(pos[:], pre[:], offs[:], ALU.add)
        big = P.sb("big", [128, NKT, 16], F32)
        P.ts(big[:], m[:], -BIG, ALU.mult, BIG, ALU.add)
        P.tt(pos[:], pos[:], m[:], ALU.mult)
        P.tt(pos[:], pos[:], big[:], ALU.add)
        posi = P.sb("posi", [128, NKT, 16], I32)
        P.copy(posi[:], pos[:])
        gf_ = P.sb("gf_", [128, NKT, 16], F32)
        gi = P.sb("gi", [128, NKT, 16], I32)
        P.ts(gf_[:], pos[:], float(ZROW), ALU.min)
        P.copy(gi[:], gf_[:])
        P.dma(S["gi"][:], gi[:])
        ixs = [P.sb(f"ix{i}", [128, 1], I32) for i in range(4)]
        ht = P.sb("ht", [128, 2, D], BF16)
        c = 0
        for j in range(NKT):
            b = j % 2
            P.dma(ht[:, b], S["h2"][j * 128:(j + 1) * 128, :])
            for e in range(16):
                ix = ixs[c % 4]
                c += 1
                P.copy(ix[:, :], posi[:, j, e:e + 1], eng="pool")

                def fn(eng, e=e, b=b, ix=ix):
                    return eng.indirect_dma_start(out=S["xsel"][e][:, :], out_offset=bass.IndirectOffsetOnAxis(ap=ix[:, :], axis=0),
                                                  in_=ht[:, b, :], in_offset=None,
                                                  bounds_check=P.bc_reg(eng, NSLOT - 1), oob_is_err=False)
                P.dma_custom("pool", fn, reads=[ht[:, b, :], ix[:, :]], writes=[S["xsel"][e][:, :]])
    with P.scope():
        identb = P.sb("identb", [128, 128], BF16)
        P.dma(identb[:], A["ident"][:], eng="pool")
        wg = P.sb("wg", [128, 16, FF], BF16)
        wu = P.sb("wu", [128, 16, FF], BF16)
        wd = P.sb("wd", [128, NFC, D], BF16)
        XT = P.sb("XT", [128, 16, 512], BF16)
        xr = P.sb("xr", [128, 2, D], BF16)
        hm = P.sb("hm", [128, NFC, NSLOT], BF16)
        sg = P.sb("sg", [128, 2, 512], F32)
        yt = P.sb("yt", [128, 2, D], BF16)
        pT = P.ps("pT", [128, 16, 128], BF16)
        pg = P.ps("pg", [128, 2, 512], F32)
        pu = P.ps("pu", [128, 2, 512], F32)
        pd = P.ps("pd", [128, 2, 512], F32)
        tcnt = 0
        gcnt = 0
        dcnt = 0
        for e in range(16):
            for q in range(4):
                P.dma(wg[:, q * 4:(q + 1) * 4], A["wg"][l, e, :, q * 4:(q + 1) * 4, :], eng="pool")
            for q in range(4):
                P.dma(wu[:, q * 4:(q + 1) * 4], A["wu"][l, e, :, q * 4:(q + 1) * 4, :], eng="pool")
            nq = max(1, NFC // 2)
            for q in range(0, NFC, nq):
                P.dma(wd[:, q:q + nq], A["wd"][l, e, :, q:q + nq, :], eng="pool")
            for (s0, n) in add_groups(NSLOT):
                for i in range(n // 128):
                    b = tcnt % 2
                    tcnt += 1
                    P.dma(xr[:, b], S["xsel"][e][s0 + i * 128:s0 + (i + 1) * 128, :])
                    for k in range(16):
                        P.transpose(pT[:, k, :], xr[:, b, k * 128:(k + 1) * 128], identb[:])
                    P.copy(XT[:, :, i * 128:(i + 1) * 128], pT[:], eng=("act" if tcnt % 2 else "dve"))
                for f in range(NFC):
                    pb = gcnt % 2
                    gcnt += 1
                    for k in range(16):
                        P.matmul(pg[:, pb, :n], wg[:, k, f * 128:(f + 1) * 128], XT[:, k, :n], start=(k == 0), stop=(k == 15))
                    for k in range(16):
                        P.matmul(pu[:, pb, :n], wu[:, k, f * 128:(f + 1) * 128], XT[:, k, :n], start=(k == 0), stop=(k == 15))
                    P.act(sg[:, pb, :n], pg[:, pb, :n], AF.Silu)
                    P.tt(hm[:, f, s0:s0 + n], sg[:, pb, :n], pu[:, pb, :n], ALU.mult)
            for st in range(NSLOT // 128):
                yb = st % 2
                for nb in range(4):
                    pb = dcnt % 2
                    dcnt += 1
                    for f in range(NFC):
                        P.matmul(pd[:, pb, :], hm[:, f, st * 128:(st + 1) * 128], wd[:, f, nb * 512:(nb + 1) * 512], start=(f == 0), stop=(f == NFC - 1))
                    P.copy(yt[:, yb, nb * 512:(nb + 1) * 512], pd[:, pb, :], eng=("act" if dcnt % 2 else "dve"))
                P.dma(S["yout"][e][st * 128:(st + 1) * 128, :], yt[:, yb])
    with P.scope():
        gate = P.sb("gate", [128, NKT, 16], F32)
        P.dma(gate[:], S["gate"][:])
        gi = P.sb("gi", [128, NKT, 16], I32)
        P.dma(gi[:], S["gi"][:])
        g2r = P.sb("g2r", [128, 2, D], F32)
        for j in range(2):
            P.dma(g2r[:, j], bcrow(A["modrow"][l, 2 + j:3 + j, :]))
        if last:
            fr = P.sb("fr", [128, D], F32)
            P.dma(fr[:], bcrow(A["fng_row"][0:1, :]))
            junk = P.sb("junk", [128, D], BF16)
            ss = P.sb("ss", [128, 2], F32)
        gt = P.sb("gt", [128, 3, D], BF16)
        acc = P.sb("acc", [128, 2, D], F32)
        xt = P.sb("xt", [128, 2, D], F32)
        ixs = [P.sb(f"ix{i}", [128, 1], I32) for i in range(4)]
        eps = P.eps_tile()
        c = 0
        for tl in range(2 if last else 0, NKT):
            b = tl % 2
            j = 1 if tl < 2 else 0
            rows = slice(tl * 128, (tl + 1) * 128)
            P.dma(xt[:, b], x_s[rows, :])
            for e in range(16):
                ix = ixs[c % 4]
                g = c % 3
                c += 1
                P.copy(ix[:, :], gi[:, tl, e:e + 1], eng="pool")

                def fn2(eng, e=e, g=g, ix=ix):
                    return eng.indirect_dma_start(out=gt[:, g, :], out_offset=None, in_=S["yout"][e][:, :],
                                                  in_offset=bass.IndirectOffsetOnAxis(ap=ix[:, :], axis=0))
                P.dma_custom("pool", fn2, reads=[S["yout"][e][:, :], ix[:, :]], writes=[gt[:, g, :]])
                if e == 0:
                    P.ts(acc[:, b], gt[:, g, :], gate[:, tl, 0:1], ALU.mult)
                else:
                    P.stt(acc[:, b], gt[:, g, :], gate[:, tl, e:e + 1], acc[:, b], ALU.mult, ALU.add)
            P.tt(acc[:, b], acc[:, b], g2r[:, j], ALU.mult)
            P.tt(acc[:, b], acc[:, b], xt[:, b], ALU.add)
            if not last:
                P.dma(x_s[rows, :], acc[:, b])
            else:
                P.act(junk[:], acc[:, b], AF.Square, accum_out=ss[:, b:b + 1])
                P.act(ss[:, b:b + 1], ss[:, b:b + 1], AF.Sqrt, bias=eps[:, :], scale=1.0 / D)
                P.recip(ss[:, b:b + 1], ss[:, b:b + 1])
                P.ts(acc[:, b], acc[:, b], ss[:, b:b + 1], ALU.mult)
                P.tt(acc[:, b], acc[:, b], fr[:], ALU.mult)
                P.dma(out_d[(tl - 2) * 128:(tl - 1) * 128, :], acc[:, b])


def phase_mod(P, A, DEPTH):
    with P.scope():
        identf = P.sb("identf", [128, 128], F32)
        P.dma(identf[:], A["ident"][:])
        c_sb = P.sb("c_sb", [128, 16, 2], F32)
        s_sb = P.sb("s_sb", [128, 16, 2], F32)
        P.dma(c_sb[:], A["cT2"][:])
        P.act(s_sb[:], c_sb[:], AF.Silu)
        wbuf = P.sb("wbuf", [128, 2, 16, 512], F32)
        bsb = P.sb("bsb", [2, 6 * D], F32)
        msb = P.sb("msb", [2, 6 * D], F32)
        mfm = P.sb("mfm", [128, 16, 8], F32)
        ps = P.ps("ps", [2, 2, 512], F32)
        pm = P.ps("pm", [128, 16, 2, 2], F32)
        P.memset(mfm[:], 0.0)
        i = 0
        for l in range(DEPTH):
            P.dma(bsb[:], A["bm"][l])
            for nb in range(6 * D // 512):
                buf = i % 2
                i += 1
                P.dma(wbuf[:, buf], A["wm"][l, :, :, nb * 512:(nb + 1) * 512])
                for k in range(16):
                    P.matmul(ps[:, buf, :], s_sb[:, k, :], wbuf[:, buf, k, :], start=(k == 0), stop=(k == 15))
                P.tt(msb[:, nb * 512:(nb + 1) * 512], ps[:, buf, :], bsb[:, nb * 512:(nb + 1) * 512], ALU.add)
            for idx, (r, seg) in enumerate([(0, 2), (1, 2), (0, 5), (1, 5), (0, 4), (0, 3), (1, 4), (1, 3)]):
                P.dma(A["modrow"][l, idx:idx + 1, :], msb[r:r + 1, seg * D:(seg + 1) * D])
            for v in range(2):
                for k in range(16):
                    P.transpose(pm[:, k, v, :], msb[0:2, v * D + k * 128:v * D + (k + 1) * 128], identf[0:2, 0:2])
            P.copy(mfm[:, :, 0], pm[:, :, 0, 0])
            P.copy(mfm[:, :, 1], pm[:, :, 1, 0])
            P.copy(mfm[:, :, 2], pm[:, :, 0, 1])
            P.copy(mfm[:, :, 3], pm[:, :, 1, 1])
            P.dma(A["modfm"][l], mfm[:])


def build_main(NL=SEQ, FF=1024, DEPTH=2, upto=None, dbg=False, final=True):
    P = Prog()
    NT = CTX + NL
    NKT = NT // 128
    KL = NL // 8
    LCAP = KL + 128
    NSLOT = LCAP + 128
    A = {}

    def inp(name, shape, dt=F32):
        A[name] = P.dram(name, shape, dt, "ExternalInput")

    inp("x_in", [NT, D])
    inp("cT2", [128, 16, 2])
    inp("wm", [DEPTH, 128, 16, 6 * D])
    inp("bm", [DEPTH, 2, 6 * D])
    A["modfm"] = P.dram("s_modfm", [DEPTH, 128, 16, 8], F32)
    A["modrow"] = P.dram("s_modrow", [DEPTH, 8, D], F32)
    inp("n1g", [DEPTH, 128, 16])
    inp("n2g_row", [DEPTH, 1, D])
    inp("ong", [DEPTH, 128, 16])
    inp("fng_row", [1, D])
    inp("w_fm", [DEPTH, FMW // 128, 128, 16 * 128])
    inp("w_tm", [DEPTH, TMW // 256, 128, 16 * 256])
    inp("kvg", [DEPTH, 128, 2])
    inp("qg", [DEPTH, 128, 4])
    inp("wuk", [DEPTH, 128, 2, 8, 128])
    inp("wuv", [DEPTH, 128, 2, 8, 128])
    inp("wqn", [DEPTH, 128, 4, 8, 128])
    inp("wqr", [DEPTH, 128, 4, 8, 2, 64])
    inp("cosT", [64, NL])
    inp("sinT", [64, NL])
    inp("natb", [DEPTH, 4, 5, 128, 576])
    inp("ident", [128, 128])
    inp("dec", [DEPTH, 128, 8])
    inp("retE", [2, 128, 128])
    inp("retM", [2, 128, 128])
    inp("retwk", [128, 2])
    inp("retwq", [128, 2, 128])
    inp("w_out", [DEPTH, 128, 16, D])
    inp("w_r", [DEPTH, 128, 16, 16])
    inp("wg", [DEPTH, 16, 128, 16, FF])
    inp("wu", [DEPTH, 16, 128, 16, FF])
    inp("wd", [DEPTH, 16, 128, FF // 128, D])
    inp("tri", [128, 128])
    out_d = P.dram("out", [NL, D], F32, "ExternalOutput") if final else None
    S = {
        "cqT": P.dram("s_cqT", [512, NT], F32), "ckvT": P.dram("s_ckvT", [256, NT], F32),
        "krT2": P.dram("s_krT2", [128, NT], F32),
        "nqT": P.dram("s_nqT", [512, NT], BF16), "nkT": P.dram("s_nkT", [512, NT], BF16),
        "rqT": P.dram("s_rqT", [512, NT], BF16), "rkT": P.dram("s_rkT", [512, NT], BF16),
        "nv": P.dram("s_nv", [NT, 512], BF16), "rk": P.dram("s_rk", [NT, 512], BF16),
        "rv": P.dram("s_rv", [NT, 512], BF16), "gfb": P.dram("s_gfb", [NT, 1024], F32),
        "mix": P.dram("s_mix", [NT, D], F32), "h2": P.dram("s_h2", [NT, D], BF16),
        "aff": P.dram("s_aff", [NT, 16], F32), "gate": P.dram("s_gate", [128, NKT, 16], F32),
        "gi": P.dram("s_gi", [128, NKT, 16], I32),
        "xsel": [P.dram(f"s_xsel{e}", [NSLOT, D], BF16) for e in range(16)],
        "yout": [P.dram(f"s_yout{e}", [NSLOT + 128, D], BF16) for e in range(16)],
    }
    x_s = P.dram("s_x", [NT, D], F32, "ExternalOutput" if (dbg or not final) else "Internal")
    if dbg:
        for k_ in ("mix", "aff", "cqT", "gfb"):
            sh_, dt_ = {"mix": ([NT, D], F32), "aff": ([NT, 16], F32), "cqT": ([512, NT], F32), "gfb": ([NT, 1024], F32)}[k_]
            S[k_] = P.dram("d_" + k_, sh_, dt_, "ExternalOutput")
    with P.scope():
        xt = P.sb("xt", [128, 2, D], F32)
        for tl in range(NKT):
            P.dma(xt[:, tl % 2], A["x_in"][tl * 128:(tl + 1) * 128, :])
            P.dma(x_s[tl * 128:(tl + 1) * 128, :], xt[:, tl % 2])
        zb = P.sb("zb", [128, D], BF16)
        zf = P.sb("zf", [128, D], F32)
        P.memset(zb[:], 0.0)
        P.memset(zf[:], 0.0)
        for e in range(16):
            for st in range(NSLOT // 128):
                P.dma(S["xsel"][e][st * 128:(st + 1) * 128, :], zb[:])
            P.dma(S["yout"][e][NSLOT:NSLOT + 128, :], zb[:])
    phase_mod(P, A, DEPTH)
    nph = 0
    for l in range(DEPTH):
        phases = [
            lambda: phase_proj2(P, NT, x_s, S, A, l),
            lambda: phase_mla(P, NT, S["cqT"], S["ckvT"], S["krT2"], S["mix"], A, l, None),
            lambda: phase_nat(P, NT, S["nqT"], S["nkT"], S["nv"], S["mix"], A["natb"], l, A["ident"]),
            lambda: phase_ret(P, NT, S["rqT"], S["rkT"], S["rk"], S["rv"], S["gfb"], S["mix"], A, l),
            lambda: phase_post(P, NT, x_s, S, A, l),
            lambda: phase_moe(P, NT, x_s, S, A, l, FF, final and l == DEPTH - 1, out_d),
        ]
        for ph in phases:
            if upto is not None and nph >= upto:
                break
            ph()
            nph += 1
    nc = P.finish()
    return nc, P


def kchunk(w):
    K, N = w.shape
    return np.ascontiguousarray(w.reshape(K // 128, 128, N).transpose(1, 0, 2))


def prep_shared(inp, NL, DEPTH):
    A = {}
    w_in = inp["w_in"]
    fm_l, tm_l = [], []
    for l in range(DEPTH):
        w = w_in[l]
        kr = w[:, 768:832]
        fm = np.concatenate([w[:, 0:512], w[:, 512:768], kr, kr[:, SW64], w[:, 832:1344], w[:, 1344:1856],
                             w[:, 2368:2880], w[:, 2880:3392]], 1)
        tm = np.concatenate([w[:, 1856:2368], w[:, 2880:3392], w[:, 3392:3904], w[:, 3904:4416], w[:, 4416:4928]], 1)
        fm_l.append(np.ascontiguousarray(kchunk(fm).reshape(128, 16, FMW // 128, 128).transpose(2, 0, 1, 3)).reshape(FMW // 128, 128, 16 * 128))
        tm_l.append(np.ascontiguousarray(kchunk(tm).reshape(128, 16, TMW // 256, 256).transpose(2, 0, 1, 3)).reshape(TMW // 256, 128, 16 * 256))
    A["w_fm"] = np.stack(fm_l)
    A["w_tm"] = np.stack(tm_l)
    A["n1g"] = np.stack([fm16(inp["norm1_g"][l]) for l in range(DEPTH)])
    A["ong"] = np.stack([fm16(inp["out_norm_g"][l]) for l in range(DEPTH)])
    A["n2g_row"] = np.ascontiguousarray(inp["norm2_g"][:DEPTH, None, :])
    A["fng_row"] = np.ascontiguousarray(inp["final_norm_g"][None, :])
    A.update(mla_weights(inp["mla_q_norm_g"][:DEPTH], inp["mla_kv_norm_g"][:DEPTH], inp["mla_w_uq"][:DEPTH],
                         inp["mla_w_uk"][:DEPTH], inp["mla_w_uv"][:DEPTH]))
    A["cosT"], A["sinT"] = rope_tables_T(NL)
    A["natb"] = np.stack([nat_bias_tables(inp["nat_rpb"][l]) for l in range(DEPTH)])
    A["ident"] = np.eye(128, dtype=np.float32)
    dec = np.concatenate([inp["ret_decay_f"][:DEPTH], inp["ret_decay_b"][:DEPTH]], 1)
    A["dec"] = np.ascontiguousarray(np.broadcast_to(dec[:, None, :], (DEPTH, 128, 8))).astype(np.float32)
    A.update(ret_consts())
    A["w_out"] = np.stack([kchunk(inp["w_out"][l]) for l in range(DEPTH)])
    A["w_r"] = np.stack([kchunk(inp["w_router"][l]) for l in range(DEPTH)])
    A["wg"] = np.stack([np.stack([kchunk(inp["w_gate"][l, e]) for e in range(16)]) for l in range(DEPTH)])
    A["wu"] = np.stack([np.stack([kchunk(inp["w_up"][l, e]) for e in range(16)]) for l in range(DEPTH)])
    A["wd"] = np.stack([np.stack([kchunk(inp["w_down"][l, e]) for e in range(16)]) for l in range(DEPTH)])
    A["tri"] = (np.arange(128)[:, None] < np.arange(128)[None, :]).astype(np.float32)
    return A


def prep_sample(x_b, ctx_b, c_b, c_ctx):
    A = {}
    A["x_in"] = np.ascontiguousarray(np.concatenate([ctx_b, x_b], 0))
    C2 = np.stack([c_b, c_ctx], 0)
    A["cT2"] = np.ascontiguousarray(C2.reshape(2, 16, 128).transpose(2, 1, 0))
    return A


def kernel(x, c, ctx, c_ctx, w_mod, b_mod, **rest):
    inp = {k: np.asarray(v, dtype=np.float32) for k, v in rest.items()}
    x = np.asarray(x, np.float32)
    ctx = np.asarray(ctx, np.float32)
    c = np.asarray(c, np.float32)
    c_ctx = np.asarray(c_ctx, np.float32)
    w_mod = np.asarray(w_mod, np.float32)
    b_mod = np.asarray(b_mod, np.float32)
    per_layer_keys = ("norm1_g", "w_in", "mla_q_norm_g", "mla_kv_norm_g", "mla_w_uq", "mla_w_uk", "mla_w_uv", "nat_rpb",
                      "ret_decay_f", "ret_decay_b", "out_norm_g", "w_out", "norm2_g", "w_router", "w_gate", "w_up", "w_down")
    x_cur = [np.concatenate([ctx[b], x[b]], 0) for b in range(NB)]
    out = None
    for l in range(DEPTH):
        final = (l == DEPTH - 1)
        inp_l = {k: (inp[k][l:l + 1] if k in per_layer_keys else inp[k]) for k in inp}
        shared = prep_shared(inp_l, SEQ, 1)
        shared["wm"] = kchunk(w_mod[l])[None]
        shared["bm"] = np.ascontiguousarray(np.broadcast_to(b_mod[l:l + 1, None, :], (1, 2, 6 * D))).astype(np.float32)
        nc, _ = build_main(SEQ, 1024, 1, final=final)
        in_maps = []
        for b in range(NB):
            m = dict(shared)
            m.update(prep_sample(x_cur[b][CTX:], x_cur[b][:CTX], c[b], c_ctx))
            in_maps.append(m)
        res = run_bass_kernel_spmd(nc, in_maps, core_ids=list(range(NB)))
        if final:
            out = np.stack([res.results[b]["out"] for b in range(NB)]).astype(np.float32)
        else:
            x_cur = [res.results[b]["s_x"] for b in range(NB)]
        del shared, in_maps
    return out
```

```python
import numpy as np
import concourse.bass as bass
import concourse.mybir as mybir
from concourse.bass_utils import run_bass_kernel_spmd

F32 = mybir.dt.float32
BF16 = mybir.dt.bfloat16
I32 = mybir.dt.int32
AF = mybir.ActivationFunctionType
ALU = mybir.AluOpType
AX = mybir.AxisListType

ENGS = ("pe", "act", "dve", "pool", "sp")
NDSEM = 12
EPS = 1e-6


def _box(ap):
    a = ap.ap
    off = int(ap.offset)
    sp = str(ap.space)
    if sp == "DRAM":
        hi = off
        for st, cn in a:
            hi += (cn - 1) * abs(st)
        return (0, 1, off, hi + 1)
    pstep = a[0][0]
    if pstep == 0:
        raise ValueError("partition-broadcast AP needs explicit box")
    p0 = off // pstep
    f0 = off % pstep
    hi = f0
    for st, cn in a[1:]:
        hi += (cn - 1) * abs(st)
    return (p0, p0 + a[0][1], f0, hi + 1)


def _ovl(b1, b2):
    return b1[0] < b2[1] and b2[0] < b1[1] and b1[2] < b2[3] and b2[2] < b1[3]


def _contains(b1, b2):
    return b1[0] <= b2[0] and b1[1] >= b2[1] and b1[2] <= b2[2] and b1[3] >= b2[3]


class Prog:
    def __init__(self):
        self.nc = bass.Bass("TRN2", target_bir_lowering=False)
        self.items = {e: [] for e in ENGS}
        self.cnt = {e: 0 for e in ENGS}
        self.dcnt = {e: 0 for e in ENGS}
        self.track = {}
        self._ctx = []
        self.n_inst = 0
        self.n_wait = {}
        self._scopes = []
        self._eps = self.sb("eps_c", [128, 1], F32)
        self.memset(self._eps[:], EPS)

    def dram(self, name, shape, dt, kind="Internal"):
        return self.nc.dram_tensor(name, list(shape), dt, kind=kind).ap()

    def sb(self, name, shape, dt):
        self._uid = getattr(self, "_uid", 0) + 1
        return self._alloc(lambda: self.nc.sbuf_tensor(f"{name}_{self._uid}", list(shape), dt))

    def ps(self, name, shape, dt=F32):
        self._uid = getattr(self, "_uid", 0) + 1
        return self._alloc(lambda: self.nc.psum_tensor(f"{name}_{self._uid}", list(shape), dt))

    def _alloc(self, mk):
        g = mk()
        t = g.__enter__()
        if self._scopes:
            self._scopes[-1].append(g)
        else:
            self._ctx.append(g)
        return t

    _scopes = []

    def scope(self):
        prog = self

        class _S:
            def __enter__(s2):
                prog._scopes = prog._scopes + [[]]
                return s2

            def __exit__(s2, *a):
                prog.barrier()
                gs = prog._scopes[-1]
                prog._scopes = prog._scopes[:-1]
                for g in reversed(gs):
                    g.__exit__(None, None, None)
                return False

        return _S()

    def barrier(self):
        deps = {}
        for e in ENGS:
            if self.cnt[e]:
                deps[e] = self.cnt[e]
            for s in range(min(NDSEM, self.dcnt[e])):
                uses = (self.dcnt[e] - 1 - s) // NDSEM + 1
                deps[("d", e, s)] = 16 * uses
        for e in ENGS:
            self.items[e].append((dict(deps), None, None))
        self.track = {}

    def _deps(self, reads, writes, tok_self):
        deps = {}

        def add(tok):
            k, v = tok
            if deps.get(k, 0) < v:
                deps[k] = v

        for ap in reads:
            bx = ap if isinstance(ap, tuple) else (ap.name, _box(ap))
            nm, b = bx
            tr = self.track.setdefault(nm, {"w": [], "r": {}})
            for (wb, tok) in tr["w"]:
                if _ovl(wb, b):
                    add(tok)
        for ap in writes:
            bx = ap if isinstance(ap, tuple) else (ap.name, _box(ap))
            nm, b = bx
            tr = self.track.setdefault(nm, {"w": [], "r": {}})
            for (wb, tok) in tr["w"]:
                if _ovl(wb, b):
                    add(tok)
            for (rb, rk), rv in tr["r"].items():
                if _ovl(rb, b):
                    add((rk, rv))
        for ap in reads:
            bx = ap if isinstance(ap, tuple) else (ap.name, _box(ap))
            nm, b = bx
            tr = self.track[nm]
            key = (b, tok_self[0])
            tr["r"][key] = tok_self[1]
        for ap in writes:
            bx = ap if isinstance(ap, tuple) else (ap.name, _box(ap))
            nm, b = bx
            tr = self.track[nm]
            tr["w"] = [(wb, t) for (wb, t) in tr["w"] if not _contains(b, wb)]
            tr["r"] = {k: v for k, v in tr["r"].items() if not _contains(b, k[0])}
            tr["w"].append((b, tok_self))
        return deps

    def op(self, eng, fn, reads=(), writes=(), pe_acc=False):
        self.cnt[eng] += 1
        tok = (eng, self.cnt[eng])
        deps = self._deps(reads, writes, tok)
        if eng == "pe":
            deps.pop("pe", None)
        self.items[eng].append((deps, fn, ("c", eng)))
        self.n_inst += 1

    def dma(self, out, in_, eng="sp", reads=None, writes=None, **kw):
        j = self.dcnt[eng]
        self.dcnt[eng] += 1
        slot = j % NDSEM
        use = j // NDSEM + 1
        key = ("d", eng, slot)
        tok = (key, 16 * use)
        deps = self._deps([in_] if reads is None else reads,
                          [out] if writes is None else writes, tok)
        if use > 1:
            if deps.get(key, 0) < 16 * (use - 1):
                deps[key] = 16 * (use - 1)

        def fn(e, out=out, in_=in_, kw=kw):
            return e.dma_start(out=out, in_=in_, **kw)

        self.items[eng].append((deps, fn, key))
        self.n_inst += 1

    def bc_reg(self, eng, val):
        if not hasattr(self, "_bcregs"):
            self._bcregs = {}
        if val not in self._bcregs:
            self._bcregs[val] = eng.to_reg(val)
        return self._bcregs[val]

    def dma_custom(self, eng, fn, reads, writes):
        j = self.dcnt[eng]
        self.dcnt[eng] += 1
        slot = j % NDSEM
        use = j // NDSEM + 1
        key = ("d", eng, slot)
        tok = (key, 16 * use)
        deps = self._deps(reads, writes, tok)
        if use > 1 and deps.get(key, 0) < 16 * (use - 1):
            deps[key] = 16 * (use - 1)
        self.items[eng].append((deps, fn, key))
        self.n_inst += 1

    def finish(self, final_waits=True):
        nc = self.nc
        sems = {}
        sem_ctx = []

        def getsem(k):
            if k not in sems:
                g = nc.semaphore("s_" + "_".join(str(x) for x in (k if isinstance(k, tuple) else (k,))))
                sems[k] = g.__enter__()
                sem_ctx.append(g)
            return sems[k]

        for e in ENGS:
            getsem(e)
        for e in ENGS:
            for s in range(min(NDSEM, self.dcnt[e])):
                getsem(("d", e, s))
        final = {}
        for e in ENGS:
            if self.cnt[e]:
                final[e] = self.cnt[e]
            for s in range(min(NDSEM, self.dcnt[e])):
                uses = (self.dcnt[e] - 1 - s) // NDSEM + 1
                final[("d", e, s)] = 16 * uses
        items = self.items
        blk = nc.Block()
        block = blk.__enter__()

        def mk(ename):
            def body(eng):
                seen = {}
                for deps, fn, inc in items[ename]:
                    for k, v in deps.items():
                        if seen.get(k, 0) < v:
                            eng.wait_ge(sems[k], v)
                            seen[k] = v
                            self.n_wait[ename] = self.n_wait.get(ename, 0) + 1
                    if fn is None:
                        continue
                    ins = fn(eng)
                    if inc[0] == "c":
                        ins.then_inc(sems[inc[1]], 1)
                    else:
                        ins.then_inc(sems[inc], 16)
                if ename == "sp" and final_waits:
                    for k, v in final.items():
                        if seen.get(k, 0) < v:
                            eng.wait_ge(sems[k], v)
            return body

        block.tensor(mk("pe"))
        block.scalar(mk("act"))
        block.vector(mk("dve"))
        block.gpsimd(mk("pool"))
        block.sync(mk("sp"))
        blk.__exit__(None, None, None)
        for g in reversed(sem_ctx):
            g.__exit__(None, None, None)
        for g in reversed(self._ctx):
            g.__exit__(None, None, None)
        return nc

    def eps_tile(self):
        return self._eps

    def matmul(self, out, lhsT, rhs, start=True, stop=True):
        self.op("pe", lambda e: e.matmul(out, lhsT, rhs, start=start, stop=stop),
                reads=[lhsT, rhs], writes=[out])

    def transpose(self, out, in_, ident):
        self.op("pe", lambda e: e.transpose(out, in_, ident), reads=[in_, ident], writes=[out])

    def act(self, out, in_, func, bias=None, scale=1.0, accum_out=None, eng="act", extra_reads=()):
        kw = {}
        rd = [in_] + list(extra_reads)
        wr = [out]
        if bias is not None:
            kw["bias"] = bias
            if not isinstance(bias, (int, float)):
                rd.append(bias)
        if not isinstance(scale, (int, float)):
            rd.append(scale)
        if accum_out is not None:
            kw["accum_out"] = accum_out
            wr.append(accum_out)
        self.op("act", lambda e: e.activation(out, in_, func, scale=scale, **kw), reads=rd, writes=wr)

    def tt(self, out, in0, in1, op, eng="dve"):
        self.op(eng, lambda e: e.tensor_tensor(out, in0, in1, op), reads=[in0, in1], writes=[out])

    def ts(self, out, in0, s1, op0, s2=None, op1=None, eng="dve", accum_out=None):
        rd = [in0]
        if not isinstance(s1, (int, float)):
            rd.append(s1)
        if s2 is not None and not isinstance(s2, (int, float)):
            rd.append(s2)
        wr = [out]
        kw = {}
        if accum_out is not None:
            kw["accum_out"] = accum_out
            wr.append(accum_out)
        if op1 is None:
            self.op(eng, lambda e: e.tensor_scalar(out, in0, s1, None, op0, **kw), reads=rd, writes=wr)
        else:
            self.op(eng, lambda e: e.tensor_scalar(out, in0, s1, s2, op0, op1, **kw), reads=rd, writes=wr)

    def stt(self, out, in0, scalar, in1, op0, op1, eng="dve", accum_out=None):
        rd = [in0, in1]
        if not isinstance(scalar, (int, float)):
            rd.append(scalar)
        wr = [out]
        kw = {}
        if accum_out is not None:
            kw["accum_out"] = accum_out
            wr.append(accum_out)
        self.op(eng, lambda e: e.scalar_tensor_tensor(out, in0, scalar, in1, op0, op1, **kw), reads=rd, writes=wr)

    def copy(self, out, in_, eng="dve"):
        if eng == "act":
            self.op("act", lambda e: e.copy(out, in_), reads=[in_], writes=[out])
        else:
            self.op(eng, lambda e: e.tensor_copy(out, in_), reads=[in_], writes=[out])

    def memset(self, ap, val, eng="dve"):
        self.op(eng, lambda e: e.memset(ap, val), reads=[], writes=[ap])

    def reduce(self, out, in_, op, axis=AX.X, eng="dve"):
        self.op(eng, lambda e: e.tensor_reduce(out, in_, axis, op), reads=[in_], writes=[out])

    def recip(self, out, in_):
        self.op("dve", lambda e: e.reciprocal(out, in_), reads=[in_], writes=[out])


D = 2048
NB = 4
SEQ = 8192
CTX = 256
DEPTH = 2
NCORE = 8
INC = 4928
EPS = 1e-6
_PROGS = {}


def _run(name, builder, in_maps):
    if name not in _PROGS:
        _PROGS[name] = builder
    nc = builder()
    res = run_bass_kernel_spmd(nc, in_maps, core_ids=list(range(NCORE)))
    return res.results


def build_mod():
    P = Prog()
    cT = P.dram("cT", [128, 16, 5], F32, "ExternalInput")
    wm = P.dram("wm", [128, 16, 2, 1536], F32, "ExternalInput")
    bm = P.dram("bm", [5, 2, 1536], F32, "ExternalInput")
    out = P.dram("mod", [5, 2, 1536], F32, "ExternalOutput")
    c_sb = P.sb("c_sb", [128, 16, 5], F32)
    s_sb = P.sb("s_sb", [128, 16, 5], F32)
    wbuf = P.sb("wbuf", [128, 2, 16, 512], F32)
    bsb = P.sb("bsb", [5, 2, 1536], F32)
    osb = P.sb("osb", [5, 2, 1536], F32)
    ps = P.ps("ps", [5, 2, 512], F32)
    P.dma(c_sb[:], cT[:])
    P.dma(bsb[:], bm[:])
    P.act(s_sb[:], c_sb[:], AF.Silu)
    i = 0
    for l in range(2):
        for nb in range(3):
            buf = i % 2
            P.dma(wbuf[:, buf], wm[:, :, l, nb * 512:(nb + 1) * 512])
            for k in range(16):
                P.matmul(ps[:, buf, :], s_sb[:, k, :], wbuf[:, buf, k, :], start=(k == 0), stop=(k == 15))
            P.tt(osb[:, l, nb * 512:(nb + 1) * 512], ps[:, buf, :], bsb[:, l, nb * 512:(nb + 1) * 512], ALU.add)
            i += 1
    P.dma(out[:], osb[:])
    return P.finish()


def run_mod(c, c_ctx, w_mod, b_mod):
    C5 = np.concatenate([c, c_ctx[None]], 0)
    cT = np.ascontiguousarray(C5.reshape(5, 16, 128).transpose(2, 1, 0))
    in_maps = []
    for i in range(NCORE):
        sl = slice(i * 1536, (i + 1) * 1536)
        wmi = np.ascontiguousarray(w_mod[:, :, sl].reshape(2, 16, 128, 1536).transpose(2, 1, 0, 3))
        bmi = np.ascontiguousarray(np.broadcast_to(b_mod[None, :, sl], (5, 2, 1536)))
        in_maps.append({"cT": cT, "wm": wmi, "bm": bmi})
    res = _run("mod", build_mod, in_maps)
    mod = np.concatenate([r["mod"] for r in res], axis=2)
    return mod


def bcast_mid(ap2, k):
    p, n = ap2.shape
    return ap2.unsqueeze(1).broadcast_to([p, k, n])


def fm_rstd(P, src, K, N, sq, ssp, ps, ones, rstd, dim):
    P.act(sq, src, AF.Square)
    if K > 1:
        P.reduce(ssp, sq.rearrange("p k t -> p t k"), ALU.add)
        red = ssp
    else:
        red = sq[:, 0, :]
    P.matmul(ps, ones, red)
    P.act(rstd, ps, AF.Sqrt, bias=P.eps_tile()[:rstd.shape[0], :], scale=1.0 / dim)
    P.recip(rstd, rstd)


NT1 = 4096 + 128


def build_proj():
    P = Prog()
    xT = P.dram("xT", [128, 16, NT1], F32, "ExternalInput")
    g1 = P.dram("g1", [128, 16], F32, "ExternalInput")
    sc = P.dram("sc", [128, 16, 2], F32, "ExternalInput")
    sh = P.dram("sh", [128, 16, 2], F32, "ExternalInput")
    w = P.dram("w", [128, 16, INC], F32, "ExternalInput")
    pT = P.dram("pT", [INC, NT1], F32, "ExternalOutput")

    hT = P.sb("hT", [128, 16, NT1], BF16)
    xs = P.sb("xs", [128, 2, 16, 128], F32)
    sq = P.sb("sq", [128, 16, 128], F32)
    ssp = P.sb("ssp", [128, 128], F32)
    rstd = P.sb("rstd", [128, 128], F32)
    ones = P.sb("ones", [128, 128], F32)
    g1s = P.sb("g1s", [128, 16], F32)
    scs = P.sb("scs", [128, 16, 2], F32)
    shs = P.sb("shs", [128, 16, 2], F32)
    G = P.sb("G", [128, 16, 2], F32)
    wb = P.sb("wb", [128, 2, 16, 128], BF16)
    ost = P.sb("ost", [128, 4, 512], F32)
    psn = P.ps("psn", [128, 128], F32)
    pso = P.ps("pso", [128, 4, 512], F32)

    P.memset(ones[:], 1.0)
    P.dma(g1s[:], g1[:])
    P.dma(scs[:], sc[:])
    P.dma(shs[:], sh[:])
    for j in range(2):
        P.stt(G[:, :, j], scs[:, :, j], 1.0, g1s[:], ALU.add, ALU.mult)
    for s in range(NT1 // 128):
        b = s % 2
        t0 = s * 128
        j = 0 if s < 32 else 1
        P.dma(xs[:, b], xT[:, :, t0:t0 + 128])
        fm_rstd(P, xs[:, b], 16, 128, sq[:], ssp[:], psn[:], ones[:], rstd[:], D)
        P.tt(sq[:], xs[:, b], bcast_mid(rstd[:], 16), ALU.mult)
        for k in range(16):
            P.act(hT[:, k, t0:t0 + 128], sq[:, k, :], AF.Identity, bias=shs[:, k, j:j + 1], scale=G[:, k, j:j + 1])
    groups = [(g * 512, 512) for g in range(8)] + [(4096, 128)]
    it = 0
    ncb = (INC + 127) // 128
    for cb in range(ncb):
        c0 = cb * 128
        ncols = min(128, INC - c0)
        wbuf = cb % 2
        P.dma(wb[:, wbuf, :, :ncols], w[:, :, c0:c0 + ncols], eng="pool")
        for (t0, n) in groups:
            pb = it % 4
            for k in range(16):
                P.matmul(pso[:ncols, pb, :n], wb[:, wbuf, k, :ncols], hT[:, k, t0:t0 + n], start=(k == 0), stop=(k == 15))
            if it % 2 == 0:
                P.copy(ost[:ncols, pb, :n], pso[:ncols, pb, :n], eng="act")
            else:
                P.copy(ost[:ncols, pb, :n], pso[:ncols, pb, :n], eng="dve")
            P.dma(pT[c0:c0 + ncols, t0:t0 + n], ost[:ncols, pb, :n])
            it += 1
    return P.finish()


def fm16(v):
    return np.ascontiguousarray(v.reshape(16, 128).T)


def to_fm(xtm):
    T = xtm.shape[0]
    return np.ascontiguousarray(xtm.reshape(T, 16, 128).transpose(2, 1, 0))


def run_proj(x_l, x_c, mod_l, norm1_g, w_in):
    wl = np.ascontiguousarray(w_in.reshape(16, 128, INC).transpose(1, 0, 2))
    g1 = fm16(norm1_g)
    in_maps = []
    for i in range(NCORE):
        b, h = i // 2, i % 2
        xt = np.concatenate([x_l[b, h * 4096:(h + 1) * 4096], x_c[b, h * 128:(h + 1) * 128]], 0)
        sc = np.stack([fm16(mod_l[b, D:2 * D]), fm16(mod_l[4, D:2 * D])], -1)
        sh = np.stack([fm16(mod_l[b, 0:D]), fm16(mod_l[4, 0:D])], -1)
        in_maps.append({"xT": to_fm(xt), "g1": g1, "sc": np.ascontiguousarray(sc), "sh": np.ascontiguousarray(sh), "w": wl})
    res = _run("proj", build_proj, in_maps)
    pT_l = np.empty((NB, INC, SEQ), np.float32)
    pT_c = np.empty((NB, INC, CTX), np.float32)
    for i in range(NCORE):
        b, h = i // 2, i % 2
        r = res[i]["pT"]
        pT_l[b, :, h * 4096:(h + 1) * 4096] = r[:, :4096]
        pT_c[b, :, h * 128:(h + 1) * 128] = r[:, 4096:]
    return pT_l, pT_c


MLA_SCALE = 192.0 ** -0.5


def add_groups(nt, width=512):
    g = []
    t = 0
    while t < nt:
        n = min(width, nt - t)
        g.append((t, n))
        t += n
    return g


def phase_mla(P, NT, cqT, ckvT, krT2, mixd, W, l, ident_d):
    NL = NT - CTX
    NKT = NT // 128
    with P.scope():
        ones = P.sb("ones", [128, 128], F32)
        P.memset(ones[:], 1.0)
        ckvn = P.sb("ckvn", [128, 2, NT], BF16)
        krr = P.sb("krr", [64, NT], BF16)
        gkv = P.sb("gkv", [128, 2], F32)
        gq = P.sb("gq", [128, 4], F32)
        P.dma(gkv[:], W["kvg"][l])
        P.dma(gq[:], W["qg"][l])
        cqn_d = P.dram(f"cqn_d{l}", [512, NT], BF16)
        st = P.sb("st", [128, 4, 512], F32)
        sq = P.sb("sq", [128, 4, 512], F32)
        ssp = P.sb("ssp", [128, 512], F32)
        rstd = P.sb("rstd", [128, 512], F32)
        psn = P.ps("psn", [128, 512], F32)
        kr2 = P.sb("kr2", [64, 2, 512], F32)
        cs = P.sb("cs", [64, 2, 512], F32)
        t1 = P.sb("t1", [64, 512], F32)
        t2 = P.sb("t2", [64, 512], F32)
        cqb = P.sb("cqb", [128, 4, 512], BF16)
        for (t0, n) in add_groups(NT):
            P.dma(st[:, :2, :n], ckvT.rearrange("(c p) t -> p c t", p=128)[:, :, t0:t0 + n])
            fm_rstd(P, st[:, :2, :n], 2, n, sq[:, :2, :n], ssp[:, :n], psn[:, :n], ones[:], rstd[:, :n], 256)
            P.tt(sq[:, :2, :n], st[:, :2, :n], bcast_mid(rstd[:, :n], 2), ALU.mult)
            for c in range(2):
                P.act(ckvn[:, c, t0:t0 + n], sq[:, c, :n], AF.Identity, scale=gkv[:, c:c + 1])
            P.dma(st[:, :, :n], cqT.rearrange("(c p) t -> p c t", p=128)[:, :, t0:t0 + n])
            fm_rstd(P, st[:, :, :n], 4, n, sq[:, :, :n], ssp[:, :n], psn[:, :n], ones[:], rstd[:, :n], 512)
            P.tt(sq[:, :, :n], st[:, :, :n], bcast_mid(rstd[:, :n], 4), ALU.mult)
            for c in range(4):
                P.act(cqb[:, c, :n], sq[:, c, :n], AF.Identity, scale=gq[:, c:c + 1])
            P.dma(cqn_d.rearrange("(c p) t -> p c t", p=128)[:, :, t0:t0 + n], cqb[:, :, :n])
            P.dma(kr2[:, :, :n], krT2.rearrange("(c p) t -> p c t", p=64)[:, :, t0:t0 + n])
            nc_ = max(0, min(n, CTX - t0))
            if nc_ > 0:
                P.copy(krr[:, t0:t0 + nc_], kr2[:, 0, :nc_])
            if nc_ < n:
                m = n - nc_
                l0 = t0 + nc_ - CTX
                P.dma(cs[:, 0, :m], W["cosT"][:, l0:l0 + m])
                P.dma(cs[:, 1, :m], W["sinT"][:, l0:l0 + m])
                P.tt(t1[:, :m], kr2[:, 0, nc_:n], cs[:, 0, :m], ALU.mult)
                P.tt(t2[:, :m], kr2[:, 1, nc_:n], cs[:, 1, :m], ALU.mult)
                P.tt(krr[:, t0 + nc_:t0 + n], t1[:, :m], t2[:, :m], ALU.add)
        wuk = P.sb("wuk", [128, 2, 128], BF16)
        wuv = P.sb("wuv", [128, 2, 128], BF16)
        wqn = P.sb("wqn", [128, 4, 128], BF16)
        wqr = P.sb("wqr", [128, 4, 2, 64], BF16)
        KhT = P.sb("KhT", [128, NT], BF16)
        Vh = P.sb("Vh", [128, NKT, 129], BF16)
        cqb2 = P.sb("cqb2", [128, 2, 4, 512], BF16)
        Qn2 = P.sb("Qn2", [128, 2, 512], BF16)
        Qr2 = P.sb("Qr2", [64, 2, 512], BF16)
        qr2b = P.sb("qr2b", [64, 2, 2, 512], F32)
        cs2 = P.sb("cs2", [64, 2, 2, 512], F32)
        PT = P.sb("PT", [128, 2, 512], BF16)
        rec = P.sb("rec", [128, 4], F32)
        osb = P.sb("osb", [128, 4, 128], F32)
        psk = P.ps("psk", [128, 512], F32)
        pss = P.ps("pss", [128, 2, 512], F32)
        acc = P.ps("acc", [128, 4, 512], F32)
        P.memset(Vh[:, :, 128:129], 1.0)
        for h in range(8):
            P.dma(wuk[:], W["wuk"][l, :, :, h, :], eng="pool")
            P.dma(wuv[:], W["wuv"][l, :, :, h, :], eng="pool")
            P.dma(wqn[:], W["wqn"][l, :, :, h, :], eng="pool")
            P.dma(wqr[:], W["wqr"][l, :, :, h, :, :], eng="pool")
            pk2 = [psk, psn]
            pi = 0
            for (t0, n) in add_groups(NT):
                pp_ = pk2[pi % 2]
                pi += 1
                for c in range(2):
                    P.matmul(pp_[:, :n], wuk[:, c, :], ckvn[:, c, t0:t0 + n], start=(c == 0), stop=(c == 1))
                P.copy(KhT[:, t0:t0 + n], pp_[:, :n], eng="act")
            for kt in range(NKT):
                pp_ = pk2[pi % 2]
                pi += 1
                for c in range(2):
                    P.matmul(pp_[:, :128], ckvn[:, c, kt * 128:(kt + 1) * 128], wuv[:, c, :], start=(c == 0), stop=(c == 1))
                P.copy(Vh[:, kt, :128], pp_[:, :128], eng="dve")
            qgroups = [(0, CTX, True)] + [(CTX + a, b, False) for (a, b) in add_groups(NL)]

            def prologue(gi):
                t0, n, isctx = qgroups[gi]
                pb = gi % 2
                P.dma(cqb2[:, pb, :, :n], cqn_d.rearrange("(c p) t -> p c t", p=128)[:, :, t0:t0 + n])
                for c in range(4):
                    P.matmul(psk[:, :n], wqn[:, c, :], cqb2[:, pb, c, :n], start=(c == 0), stop=(c == 3))
                P.copy(Qn2[:, pb, :n], psk[:, :n], eng="act")
                for j in range(2):
                    pj = psn if j == 0 else psk
                    for c in range(4):
                        P.matmul(pj[:64, :n], wqr[:, c, j, :], cqb2[:, pb, c, :n], start=(c == 0), stop=(c == 3))
                    P.copy(qr2b[:, pb, j, :n], pj[:64, :n], eng="dve")
                    if isctx:
                        break
                if isctx:
                    P.copy(Qr2[:, pb, :n], qr2b[:, pb, 0, :n])
                else:
                    l0 = t0 - CTX
                    P.dma(cs2[:, pb, 0, :n], W["cosT"][:, l0:l0 + n])
                    P.dma(cs2[:, pb, 1, :n], W["sinT"][:, l0:l0 + n])
                    P.tt(t1[:, :n], qr2b[:, pb, 0, :n], cs2[:, pb, 0, :n], ALU.mult)
                    P.tt(t2[:, :n], qr2b[:, pb, 1, :n], cs2[:, pb, 1, :n], ALU.mult)
                    P.tt(Qr2[:, pb, :n], t1[:, :n], t2[:, :n], ALU.add)

            prologue(0)
            for gi, (t0, n, isctx) in enumerate(qgroups):
                if gi + 1 < len(qgroups):
                    prologue(gi + 1)
                pb = gi % 2
                kts = range(CTX // 128) if isctx else range(NKT)
                nq = n // 128
                last = len(kts) - 1
                for i, kt in enumerate(kts):
                    b = i % 2
                    P.matmul(pss[:, b, :n], KhT[:, kt * 128:(kt + 1) * 128], Qn2[:, pb, :n], start=True, stop=False)
                    P.matmul(pss[:, b, :n], krr[:, kt * 128:(kt + 1) * 128], Qr2[:, pb, :n], start=False, stop=True)
                    P.act(PT[:, b, :n], pss[:, b, :n], AF.Exp, scale=MLA_SCALE)
                    for qb in range(nq):
                        P.matmul(acc[:, qb, :129], PT[:, b, qb * 128:(qb + 1) * 128], Vh[:, kt, :], start=(i == 0), stop=(i == last))
                for qb in range(nq):
                    P.recip(rec[:, qb:qb + 1], acc[:, qb, 128:129])
                    P.ts(osb[:, qb, :], acc[:, qb, :128], rec[:, qb:qb + 1], ALU.mult)
                P.dma(mixd[t0:t0 + n, h * 128:(h + 1) * 128].rearrange("(q p) d -> p q d", p=128), osb[:, :nq, :])


def rope_tables_T(n):
    t = np.arange(n)
    inv = 10000.0 ** (-np.arange(16, dtype=np.float32) / 16)
    ang_r = (t // 64).astype(np.float32)[:, None] * inv
    ang_c = (t % 64).astype(np.float32)[:, None] * inv
    cosT = np.concatenate([np.cos(ang_r), np.cos(ang_r), np.cos(ang_c), np.cos(ang_c)], 1).T
    sinT = np.concatenate([-np.sin(ang_r), np.sin(ang_r), -np.sin(ang_c), np.sin(ang_c)], 1).T
    return np.ascontiguousarray(cosT.astype(np.float32)), np.ascontiguousarray(sinT.astype(np.float32))


SW64 = np.array([i + 16 if (i % 32) < 16 else i - 16 for i in range(64)])


def mla_weights(mla_q_norm_g, mla_kv_norm_g, mla_w_uq, mla_w_uk, mla_w_uv):
    L = mla_w_uq.shape[0]
    W = {}
    W["kvg"] = np.ascontiguousarray(mla_kv_norm_g.reshape(L, 2, 128).transpose(0, 2, 1))
    W["qg"] = np.ascontiguousarray(mla_q_norm_g.reshape(L, 4, 128).transpose(0, 2, 1))
    W["wuk"] = np.ascontiguousarray(mla_w_uk.reshape(L, 2, 128, 8, 128).transpose(0, 2, 1, 3, 4))
    W["wuv"] = np.ascontiguousarray(mla_w_uv.reshape(L, 2, 128, 8, 128).transpose(0, 2, 1, 3, 4))
    uq = mla_w_uq.reshape(L, 4, 128, 8, 192).transpose(0, 2, 1, 3, 4)
    W["wqn"] = np.ascontiguousarray(uq[..., :128])
    r = uq[..., 128:]
    W["wqr"] = np.ascontiguousarray(np.stack([r, r[..., SW64]], axis=-2))
    return W


NAT_SCALE = 128.0 ** -0.5
NEG = -30000.0


def nat_case(r, NR):
    if r == 0:
        return 0, 0, 9
    if r == 2:
        return 1, 0, 9
    if r == NR - 4:
        return 3, NR - 8, 8
    if r == NR - 2:
        return 4, NR - 8, 8
    return 2, r - 4, 9


def nat_bias_tables(rpb):
    H = rpb.shape[0]
    out = np.full((H, 5, 128, 576), NEG, np.float32)
    cases = [((7, 0), (6, 0)), ((5, 0), (4, 0)), ((3, 0), (2, 1)), ((3, 0), (2, 0)), ((1, 0), (0, 0))]
    qc = np.arange(64)
    c0 = np.clip(qc - 8, 0, 48)
    for ci, cs in enumerate(cases):
        for qr in range(2):
            off, i0 = cs[qr]
            for i in range(i0, i0 + 8):
                dr = i + off
                for q in range(64):
                    kc = np.arange(c0[q], c0[q] + 16)
                    out[:, ci, qr * 64 + q, i * 64 + kc] = rpb[:, dr, kc - q + 15]
    return out


def phase_nat(P, NT, nqT, nkT, nv, mixd, natb, l, identb_d):
    NL = NT - CTX
    NR = NL // 64
    with P.scope():
        ident = P.sb("identb", [128, 128], BF16)
        P.dma(ident[:], identb_d[:], eng="pool")
        KT = P.sb("KT", [128, NT], BF16)
        QT = P.sb("QT", [128, NT], BF16)
        Vrow = P.sb("Vrow", [64, NR, 128], BF16)
        Vctx = P.sb("Vctx", [128, 2, 128], BF16)
        bias = P.sb("bias", [128, 5, 576], BF16)
        Pb = P.sb("Pb", [128, 2, 832], BF16)
        PT1 = P.sb("PT1", [64, 2, 8, 128], BF16)
        PT2 = P.sb("PT2", [128, 2, 3, 128], BF16)
        mx = P.sb("mx", [128, 2], F32)
        rs = P.sb("rs", [128, 2], F32)
        osb = P.sb("osb", [128, 2, 128], F32)
        S = P.ps("S", [128, 2, 1024], F32)
        pt1 = P.ps("pt1", [64, 8, 128], BF16)
        pt2 = P.ps("pt2", [128, 3, 128], BF16)
        O = P.ps("O", [128, 2, 128], F32)
        for h in range(4):
            hs = slice(h * 128, (h + 1) * 128)
            P.dma(KT[:], nkT[hs, :])
            P.dma(QT[:], nqT[hs, :])
            P.act(QT[:], QT[:], AF.Copy, scale=NAT_SCALE)
            P.dma(Vrow[:], nv[CTX:, hs].rearrange("(r c) d -> c r d", c=64))
            P.dma(Vctx[:], nv[0:CTX, hs].rearrange("(j p) d -> p j d", p=128))
            P.dma(bias[:], natb[l, h].rearrange("c p k -> p c k"), eng="pool")
            units = [("ctx", qt) for qt in range(2)] + [("lat", pr) for pr in range(NR // 2)]
            for ui, (kind, idx) in enumerate(units):
                b = ui % 2
                if kind == "ctx":
                    q0 = idx * 128
                    nrows = 0
                    woff = 0
                    P.matmul(S[:, b, 0:256], QT[:, q0:q0 + 128], KT[:, 0:CTX], start=True, stop=True)
                else:
                    r = idx * 2
                    ci, R0, nrows = nat_case(r, NR)
                    q0 = CTX + r * 64
                    k0 = CTX + R0 * 64
                    woff = nrows * 64
                    P.matmul(S[:, b, 0:512], QT[:, q0:q0 + 128], KT[:, k0:k0 + 512], start=True, stop=False)
                    P.matmul(S[:, b, 0:512], ident[:], bias[:, ci, 0:512], start=False, stop=True)
                    if nrows == 9:
                        P.matmul(S[:, b, 512:576], QT[:, q0:q0 + 128], KT[:, k0 + 512:k0 + 576], start=True, stop=False)
                        P.matmul(S[:, b, 512:576], ident[:], bias[:, ci, 512:576], start=False, stop=True)
                    P.matmul(S[:, b, woff:woff + 256], QT[:, q0:q0 + 128], KT[:, 0:CTX], start=True, stop=True)
                tot = woff + 256
                P.reduce(mx[:, b:b + 1], S[:, b, :tot], ALU.max)
                P.ts(mx[:, b:b + 1], mx[:, b:b + 1], -1.0, ALU.mult)
                P.act(Pb[:, b, :tot], S[:, b, :tot], AF.Exp, bias=mx[:, b:b + 1], scale=1.0, accum_out=rs[:, b:b + 1])
                for i in range(min(nrows, 8)):
                    P.transpose(pt1[:, i, :], Pb[:, b, i * 64:(i + 1) * 64], ident[:])
                if nrows == 9:
                    P.transpose(pt2[:64, 0, :], Pb[:, b, 512:576], ident[:])
                for j in range(2):
                    P.transpose(pt2[:, 1 + j, :], Pb[:, b, woff + j * 128:woff + (j + 1) * 128], ident[:])
                if nrows:
                    P.copy(PT1[:, b], pt1[:], eng="act")
                P.copy(PT2[:, b], pt2[:], eng="dve")
                mms = []
                for i in range(min(nrows, 8)):
                    mms.append((PT1[:, b, i, :], Vrow[:, R0 + i, :]))
                if nrows == 9:
                    mms.append((PT2[:64, b, 0, :], Vrow[:, R0 + 8, :]))
                for j in range(2):
                    mms.append((PT2[:, b, 1 + j, :], Vctx[:, j, :]))
                for mi, (lt, rh) in enumerate(mms):
                    P.matmul(O[:, b, :], lt, rh, start=(mi == 0), stop=(mi == len(mms) - 1))
                P.recip(rs[:, b:b + 1], rs[:, b:b + 1])
                P.ts(osb[:, b, :], O[:, b, :], rs[:, b:b + 1], ALU.mult)
                P.dma(mixd[q0:q0 + 128, 1024 + h * 128:1024 + (h + 1) * 128], osb[:, b, :])


def ret_consts():
    s = np.arange(128)[:, None].astype(np.float32)
    c = np.arange(128)[None, :].astype(np.float32)
    E = np.stack([np.maximum(c - s, 0), np.maximum(s - c, 0)]).astype(np.float32)
    M = np.stack([(c >= s), (s >= c)]).astype(np.float32)
    p = np.arange(128).astype(np.float32)
    wkexp = np.stack([127 - p, p], 1).astype(np.float32)
    wq = np.stack([np.arange(128) + 1.0, 128.0 - np.arange(128)]).astype(np.float32)
    wqexp = np.ascontiguousarray(np.broadcast_to(wq[None], (128, 2, 128))).astype(np.float32)
    return {"retE": E, "retM": M, "retwk": wkexp, "retwq": wqexp}


def phase_ret(P, NT, rqT, rkT, rk, rv, gfb, mixd, C, l):
    NKT = NT // 128
    with P.scope():
        dec = P.sb("dec", [128, 8], F32)
        lg = P.sb("lg", [128, 8], F32)
        gch = P.sb("gch", [128, 8], F32)
        E = P.sb("E", [128, 2, 128], F32)
        M = P.sb("M", [128, 2, 128], F32)
        wke = P.sb("wke", [128, 2], F32)
        wqe = P.sb("wqe", [128, 2, 128], F32)
        P.dma(dec[:], C["dec"][l])
        P.dma(E[:], C["retE"].rearrange("d s c -> s d c"))
        P.dma(M[:], C["retM"].rearrange("d s c -> s d c"))
        P.dma(wke[:], C["retwk"][:])
        P.dma(wqe[:], C["retwq"][:])
        P.act(lg[:], dec[:], AF.Sigmoid)
        P.act(lg[:], lg[:], AF.Ln)
        P.act(gch[:], lg[:], AF.Exp, scale=128.0)
        dmT = P.sb("dmT", [128, 128], F32)
        wk = P.sb("wk", [128, 1], F32)
        wqr = P.sb("wqr", [128, 128], F32)
        KT = P.sb("KT", [128, NT], BF16)
        QT = P.sb("QT", [128, NT], BF16)
        Ktm = P.sb("Ktm", [128, NKT, 128], BF16)
        Vtm = P.sb("Vtm", [128, NKT, 128], BF16)
        rbuf = P.sb("rbuf", [128, NKT, 128], F32)
        Sf = P.sb("Sf", [128, 128], F32)
        Sb = P.sb("Sb", [128, 128], BF16)
        kw = P.sb("kw", [128, 2, 128], BF16)
        sTm = P.sb("sTm", [128, 2, 128], BF16)
        Qw = P.sb("Qw", [128, 2, 128], BF16)
        gt = P.sb("gt", [128, 2, 128], F32)
        ocp = P.sb("ocp", [128, 2, 128], F32)
        junk = P.sb("junk", [128, 128], F32)
        st = P.sb("st", [128, 2, 8], F32)
        pkv = P.ps("pkv", [128, 2, 128], F32)
        psT = P.ps("psT", [128, 2, 128], F32)
        po = P.ps("po", [128, 2, 128], F32)
        fwd = list(range(NKT))
        bwd = [1, 0] + list(range(NKT - 1, 1, -1))
        for h in range(4):
            hs = slice(h * 128, (h + 1) * 128)
            P.dma(KT[:], rkT[hs, :])
            P.dma(QT[:], rqT[hs, :])
            P.dma(Ktm[:], rk[:, hs].rearrange("(j p) d -> p j d", p=128))
            P.dma(Vtm[:], rv[:, hs].rearrange("(j p) d -> p j d", p=128))
            for d in range(2):
                hd = d * 4 + h
                P.act(dmT[:], E[:, d, :], AF.Exp, scale=lg[:, hd:hd + 1])
                P.tt(dmT[:], dmT[:], M[:, d, :], ALU.mult)
                P.act(wk[:], wke[:, d:d + 1], AF.Exp, scale=lg[:, hd:hd + 1])
                P.act(wqr[:], wqe[:, d, :], AF.Exp, scale=lg[:, hd:hd + 1])
                P.memset(Sf[:], 0.0)
                P.memset(Sb[:], 0.0)
                for ci, tt in enumerate(fwd if d == 0 else bwd):
                    b = ci % 2
                    ts_ = slice(tt * 128, (tt + 1) * 128)
                    P.ts(kw[:, b, :], Ktm[:, tt, :], wk[:, 0:1], ALU.mult)
                    P.matmul(pkv[:, b, :], kw[:, b, :], Vtm[:, tt, :])
                    P.matmul(psT[:, b, :], KT[:, ts_], QT[:, ts_])
                    P.tt(sTm[:, b, :], psT[:, b, :], dmT[:], ALU.mult)
                    P.tt(Qw[:, b, :], QT[:, ts_], wqr[:], ALU.mult, eng="pool")
                    P.matmul(po[:, b, :], sTm[:, b, :], Vtm[:, tt, :], start=True, stop=False)
                    P.matmul(po[:, b, :], Qw[:, b, :], Sb[:], start=False, stop=True)
                    P.stt(Sf[:], Sf[:], gch[:, hd:hd + 1], pkv[:, b, :], ALU.mult, ALU.add)
                    P.copy(Sb[:], Sf[:], eng="act")
                    P.dma(gt[:, b, :], gfb[ts_, d * 512 + h * 128:d * 512 + (h + 1) * 128])
                    s = st[:, b, :]
                    P.act(ocp[:, b, :], po[:, b, :], AF.Identity, accum_out=s[:, 0:1])
                    P.act(junk[:], po[:, b, :], AF.Square, accum_out=s[:, 1:2])
                    P.ts(s[:, 2:3], s[:, 0:1], 1.0 / 128, ALU.mult)
                    P.tt(s[:, 3:4], s[:, 2:3], s[:, 2:3], ALU.mult)
                    P.stt(s[:, 4:5], s[:, 1:2], 1.0 / 128, s[:, 3:4], ALU.mult, ALU.subtract)
                    P.act(s[:, 5:6], s[:, 4:5], AF.Sqrt, bias=P.eps_tile()[:, :], scale=1.0)
                    P.recip(s[:, 5:6], s[:, 5:6])
                    P.ts(ocp[:, b, :], ocp[:, b, :], s[:, 2:3], ALU.subtract, s[:, 5:6], ALU.mult)
                    P.act(gt[:, b, :], gt[:, b, :], AF.Silu)
                    if d == 0:
                        P.tt(rbuf[:, tt, :], gt[:, b, :], ocp[:, b, :], ALU.mult)
                    else:
                        P.tt(ocp[:, b, :], gt[:, b, :], ocp[:, b, :], ALU.mult)
                        P.tt(ocp[:, b, :], ocp[:, b, :], rbuf[:, tt, :], ALU.add)
                        P.dma(mixd[ts_, 1536 + h * 128:1536 + (h + 1) * 128], ocp[:, b, :])


RET_KS = 128.0 ** -0.5
FMW = 2944
TMW = 2560


def bcrow(row):
    n = row.shape[-1]
    return bass.AP(row.tensor, row.offset, [[0, 128], [1, n]])


def phase_proj2(P, NT, x_s, S, A, l, parts=(1, 1, 1)):
    NKT = NT // 128
    with P.scope():
        identb = P.sb("identb", [128, 128], BF16)
        P.dma(identb[:], A["ident"][:], eng="pool")
        mf = P.sb("mf", [128, 16, 8], F32)
        P.dma(mf[:], A["modfm"][l])
        n1 = P.sb("n1", [128, 16], F32)
        P.dma(n1[:], A["n1g"][l])
        G = P.sb("G", [128, 16, 2], F32)
        for j in range(2):
            P.stt(G[:, :, j], mf[:, :, 1 + 2 * j], 1.0, n1[:], ALU.add, ALU.mult)
        hT = P.sb("hT", [128, 16, 18 * 128], BF16)
        xt = P.sb("xt", [128, 2, D], F32)
        xn = P.sb("xn", [128, 2, D], BF16)
        ss = P.sb("ss", [128, 2], F32)
        wf = P.sb("wf", [128, 2, 16, 128], BF16)
        wt = P.sb("wt", [128, 2, 16, 256], BF16)
        oF = P.sb("oF", [128, 2, 512], F32)
        oB = P.sb("oB", [128, 2, 512], BF16)
        pt = P.ps("pt", [128, 16, 128], BF16)
        pso = P.ps("pso", [128, 2, 512], F32)
        tmpT = P.sb("tmpT", [128, 16, 128], F32)
        eps = P.eps_tile()
        chunks = []
        t = 0
        while t < NKT:
            n = min(18 if t == 0 else 16, NKT - t)
            chunks.append((t, n))
            t += n
        it = 0
        wi = 0
        for (tl0, ntl) in chunks:
            for i in range(ntl):
                tl = tl0 + i
                b = tl % 2
                j = 1 if tl < 2 else 0
                P.dma(xt[:, b], x_s[tl * 128:(tl + 1) * 128, :])
                P.act(xn[:, b], xt[:, b], AF.Square, accum_out=ss[:, b:b + 1])
                P.act(ss[:, b:b + 1], ss[:, b:b + 1], AF.Sqrt, bias=eps[:, :], scale=1.0 / D)
                P.recip(ss[:, b:b + 1], ss[:, b:b + 1])
                P.ts(xn[:, b], xt[:, b], ss[:, b:b + 1], ALU.mult)
                for k in range(16):
                    P.transpose(pt[:, k, :], xn[:, b, k * 128:(k + 1) * 128], identb[:])
                P.copy(tmpT[:], pt[:], eng="act")
                P.tt(tmpT[:], tmpT[:], G[:, :, j].unsqueeze(2).broadcast_to([128, 16, 128]), ALU.mult, eng="pool")
                P.tt(hT[:, :, i * 128:(i + 1) * 128], tmpT[:], mf[:, :, 2 * j].unsqueeze(2).broadcast_to([128, 16, 128]), ALU.add)
            groups = []
            g0 = 0
            if tl0 == 0:
                groups.append((0, 2))
                g0 = 2
            while g0 < ntl:
                gn = min(4, ntl - g0)
                groups.append((g0, gn))
                g0 += gn
            for cb in range(FMW // 128 if parts[1] else 0):
                wb = wi % 2
                wi += 1
                P.dma(wf[:, wb].rearrange("p k n -> p (k n)"), A["w_fm"][l, cb], eng="pool")
                if cb < 4:
                    dest, r0, isbf, scl = S["cqT"], cb * 128, False, 1.0
                elif cb < 6:
                    dest, r0, isbf, scl = S["ckvT"], (cb - 4) * 128, False, 1.0
                elif cb == 6:
                    dest, r0, isbf, scl = S["krT2"], 0, False, 1.0
                elif cb < 11:
                    dest, r0, isbf, scl = S["nqT"], (cb - 7) * 128, True, 1.0
                elif cb < 15:
                    dest, r0, isbf, scl = S["nkT"], (cb - 11) * 128, True, 1.0
                elif cb < 19:
                    dest, r0, isbf, scl = S["rqT"], (cb - 15) * 128, True, 1.0
                else:
                    dest, r0, isbf, scl = S["rkT"], (cb - 19) * 128, True, RET_KS
                for (g0, gn) in groups:
                    n = gn * 128
                    c0 = g0 * 128
                    tok0 = (tl0 + g0) * 128
                    pb = it % 2
                    for k in range(16):
                        P.matmul(pso[:, pb, :n], wf[:, wb, k, :], hT[:, k, c0:c0 + n], start=(k == 0), stop=(k == 15))
                    stage = oB if isbf else oF
                    if scl != 1.0:
                        P.act(stage[:, pb, :n], pso[:, pb, :n], AF.Copy, scale=scl)
                    else:
                        P.copy(stage[:, pb, :n], pso[:, pb, :n], eng=("act" if it % 2 == 0 else "dve"))
                    P.dma(dest[r0:r0 + 128, tok0:tok0 + n], stage[:, pb, :n])
                    it += 1
            for tb in range(TMW // 256 if parts[2] else 0):
                wb = wi % 2
                wi += 1
                P.dma(wt[:, wb].rearrange("p k n -> p (k n)"), A["w_tm"][l, tb], eng="pool")
                if tb < 2:
                    dest, c0, isbf, scl = S["nv"], tb * 256, True, 1.0
                elif tb < 4:
                    dest, c0, isbf, scl = S["rk"], (tb - 2) * 256, True, RET_KS
                elif tb < 6:
                    dest, c0, isbf, scl = S["rv"], (tb - 4) * 256, True, 1.0
                else:
                    dest, c0, isbf, scl = S["gfb"], (tb - 6) * 256, False, 1.0
                for i in range(ntl):
                    tl = tl0 + i
                    pb = it % 2
                    for k in range(16):
                        P.matmul(pso[:, pb, :256], hT[:, k, i * 128:(i + 1) * 128], wt[:, wb, k, :], start=(k == 0), stop=(k == 15))
                    stage = oB if isbf else oF
                    if scl != 1.0:
                        P.act(stage[:, pb, :256], pso[:, pb, :256], AF.Copy, scale=scl)
                    else:
                        P.copy(stage[:, pb, :256], pso[:, pb, :256], eng=("act" if it % 2 == 0 else "dve"))
                    P.dma(dest[tl * 128:(tl + 1) * 128, c0:c0 + 256], stage[:, pb, :256])
                    it += 1


def phase_post(P, NT, x_s, S, A, l):
    NKT = NT // 128
    with P.scope():
        identb = P.sb("identb", [128, 128], BF16)
        P.dma(identb[:], A["ident"][:], eng="pool")
        identf = P.sb("identf", [128, 128], F32)
        P.dma(identf[:], A["ident"][:])
        wo = P.sb("wo", [128, 16, D], BF16)
        for q in range(4):
            P.dma(wo[:, q * 4:(q + 1) * 4].rearrange("p k f -> p (k f)"), A["w_out"][l, :, q * 4:(q + 1) * 4, :].rearrange("p k f -> p (k f)"), eng="pool")
        wr = P.sb("wr", [128, 16, 16], F32)
        P.dma(wr[:], A["w_r"][l])
        ong = P.sb("ong", [128, 16], F32)
        P.dma(ong[:], A["ong"][l])
        g1r = P.sb("g1r", [128, 2, D], F32)
        G2r = P.sb("G2r", [128, 2, D], F32)
        S2r = P.sb("S2r", [128, 2, D], F32)
        n2r = P.sb("n2r", [128, D], F32)
        P.dma(n2r[:], bcrow(A["n2g_row"][l]))
        for j in range(2):
            P.dma(g1r[:, j], bcrow(A["modrow"][l, j:j + 1, :]))
            P.dma(G2r[:, j], bcrow(A["modrow"][l, 4 + 2 * j:5 + 2 * j, :]))
            P.dma(S2r[:, j], bcrow(A["modrow"][l, 5 + 2 * j:6 + 2 * j, :]))
            P.stt(G2r[:, j], G2r[:, j], 1.0, n2r[:], ALU.add, ALU.mult)
        mx = P.sb("mx", [128, D], F32)
        xt = P.sb("xt", [128, D], F32)
        junk = P.sb("junk", [128, D], BF16)
        yn = P.sb("yn", [128, D], BF16)
        ynT = P.sb("ynT", [128, 16, 128], BF16)
        xnew = P.sb("xnew", [128, D], F32)
        h2f = P.sb("h2f", [128, D], F32)
        h2b = P.sb("h2b", [128, D], BF16)
        h2T = P.sb("h2T", [128, 16, 128], F32)
        s3 = P.sb("s3", [128, 8], F32)
        ex = P.sb("ex", [128, 16], F32)
        af = P.sb("af", [128, 16], F32)
        psT = P.ps("psT", [128, 16, 128], BF16)
        tmpT = P.sb("tmpT", [128, 16, 128], F32)
        psA = P.ps("psA", [128, 4, 512], F32)
        psr = P.ps("psr", [128, 16], F32)
        eps = P.eps_tile()
        psA_flat = psA[:].rearrange("p a b -> p (a b)")
        psA_t = psA[:].rearrange("p a (c d) -> p (a c) d", d=128)
        grp = [(0, 1024), (1024, 1536), (1536, 2048)]
        for tl in range(NKT):
            rows = slice(tl * 128, (tl + 1) * 128)
            j = 1 if tl < 2 else 0
            P.dma(mx[:], S["mix"][rows, :])
            P.dma(xt[:], x_s[rows, :])
            for i, (a0, a1) in enumerate(grp):
                P.act(junk[:, a0:a1], mx[:, a0:a1], AF.Square, accum_out=s3[:, i:i + 1])
                P.act(s3[:, i:i + 1], s3[:, i:i + 1], AF.Sqrt, bias=eps[:, :], scale=1.0 / (a1 - a0))
            P.recip(s3[:, 0:3], s3[:, 0:3])
            for i, (a0, a1) in enumerate(grp):
                P.ts(yn[:, a0:a1], mx[:, a0:a1], s3[:, i:i + 1], ALU.mult)
            for k in range(16):
                P.transpose(psT[:, k, :], yn[:, k * 128:(k + 1) * 128], identb[:])
            P.copy(tmpT[:], psT[:], eng="act")
            P.tt(ynT[:], tmpT[:], ong[:].unsqueeze(2).broadcast_to([128, 16, 128]), ALU.mult, eng="pool")
            for nb in range(4):
                for k in range(16):
                    P.matmul(psA[:, nb, :], ynT[:, k, :], wo[:, k, nb * 512:(nb + 1) * 512], start=(k == 0), stop=(k == 15))
            P.tt(xnew[:], psA_flat, g1r[:, j], ALU.mult)
            P.tt(xnew[:], xnew[:], xt[:], ALU.add)
            P.dma(x_s[rows, :], xnew[:])
            P.act(junk[:], xnew[:], AF.Square, accum_out=s3[:, 3:4])
            P.act(s3[:, 3:4], s3[:, 3:4], AF.Sqrt, bias=eps[:, :], scale=1.0 / D)
            P.recip(s3[:, 3:4], s3[:, 3:4])
            P.ts(h2f[:], xnew[:], s3[:, 3:4], ALU.mult)
            P.tt(h2f[:], h2f[:], G2r[:, j], ALU.mult)
            P.tt(h2f[:], h2f[:], S2r[:, j], ALU.add)
            P.copy(h2b[:], h2f[:], eng="pool")
            P.dma(S["h2"][rows, :], h2b[:])
            for k in range(16):
                P.transpose(psA_t[:, k, :], h2f[:, k * 128:(k + 1) * 128], identf[:])
            P.copy(h2T[:], psA_t, eng="act")
            for k in range(16):
                P.matmul(psr[:], h2T[:, k, :], wr[:, k, :], start=(k == 0), stop=(k == 15))
            P.reduce(s3[:, 4:5], psr[:], ALU.max)
            P.ts(s3[:, 4:5], s3[:, 4:5], -1.0, ALU.mult)
            P.act(ex[:], psr[:], AF.Exp, bias=s3[:, 4:5], scale=1.0, accum_out=s3[:, 5:6])
            P.recip(s3[:, 5:6], s3[:, 5:6])
            P.ts(af[:], ex[:], s3[:, 5:6], ALU.mult)
            P.dma(S["aff"][rows, :], af[:])


def phase_moe(P, NT, x_s, S, A, l, FF, last, out_d):
    NKT = NT // 128
    NL = NT - CTX
    NJL = NKT - 2
    KL = NL // 8
    KC = CTX // 8
    LCAP = KL + 128
    NSLOT = LCAP + 128
    ZROW = NSLOT
    NFC = FF // 128
    BIG = 1.0e6
    with P.scope():
        a2 = P.sb("a2", [128, NKT, 16], F32)
        P.dma(a2[:], S["aff"].rearrange("(j p) e -> p j e", p=128))
        ones = P.sb("ones", [128, 128], F32)
        P.memset(ones[:], 1.0)
        tri = P.sb("tri", [128, 128], F32)
        P.dma(tri[:], A["tri"][:])
        m = P.sb("m", [128, NKT, 16], F32)
        pc = P.ps("pc", [128, 16], F32)
        for (j0, nj, kcap) in ((2, NJL, KL), (0, 2, KC)):
            a = P.sb("a", [128, 16, nj], F32)
            P.copy(a[:], a2[:, j0:j0 + nj, :].rearrange("p j e -> p e j"))
            lo = P.sb("lo", [128, 16], F32)
            hi = P.sb("hi", [128, 16], F32)
            mid = P.sb("mid", [128, 16], F32)
            ge = P.sb("ge", [128, 16], F32)
            dlt = P.sb("dlt", [128, 16], F32)
            msk = P.sb("msk", [128, 16, nj], F32)
            cntp = P.sb("cntp", [128, 16], F32)
            P.memset(lo[:], 0.0)
            P.memset(hi[:], 1.0)
            for it in range(34):
                P.tt(mid[:], lo[:], hi[:], ALU.add)
                P.ts(mid[:], mid[:], 0.5, ALU.mult)
                P.tt(msk[:], a[:], mid[:].unsqueeze(2).broadcast_to([128, 16, nj]), ALU.is_ge)
                P.reduce(cntp[:], msk[:], ALU.add)
                P.matmul(pc[:], ones[:], cntp[:])
                P.ts(ge[:], pc[:], float(kcap), ALU.is_ge)
                P.tt(dlt[:], mid[:], lo[:], ALU.subtract)
                P.tt(dlt[:], dlt[:], ge[:], ALU.mult)
                P.tt(lo[:], lo[:], dlt[:], ALU.add)
                P.tt(dlt[:], hi[:], mid[:], ALU.subtract)
                P.tt(dlt[:], dlt[:], ge[:], ALU.mult)
                P.tt(hi[:], mid[:], dlt[:], ALU.add)
            P.tt(m[:, j0:j0 + nj, :], a2[:, j0:j0 + nj, :], lo[:].unsqueeze(1).broadcast_to([128, nj, 16]), ALU.is_ge)
        gate = P.sb("gate", [128, NKT, 16], F32)
        P.tt(gate[:], a2[:], m[:], ALU.mult)
        P.dma(S["gate"][:], gate[:])
        pre = P.sb("pre", [128, NKT, 16], F32)
        tot = P.sb("tot", [128, NKT, 16], F32)
        pp = P.ps("pp", [128, 512], F32)
        ptt = P.ps("ptt", [128, 512], F32)
        m2d = m[:].rearrange("p j e -> p (j e)")
        pre2d = pre[:].rearrange("p j e -> p (j e)")
        tot2d = tot[:].rearrange("p j e -> p (j e)")
        for c0 in range(0, NKT * 16, 512):
            n = min(512, NKT * 16 - c0)
            P.matmul(pp[:, :n], tri[:], m2d[:, c0:c0 + n])
            P.matmul(ptt[:, :n], ones[:], m2d[:, c0:c0 + n])
            P.copy(pre2d[:, c0:c0 + n], pp[:, :n], eng="act")
            P.copy(tot2d[:, c0:c0 + n], ptt[:, :n], eng="dve")
        offs = P.sb("offs", [128, NKT, 16], F32)
        P.memset(offs[:, 0, :], float(LCAP))
        P.tt(offs[:, 1, :], offs[:, 0, :], tot[:, 0, :], ALU.add)
        P.memset(offs[:, 2, :], 0.0)
        for j in range(3, NKT):
            P.tt(offs[:, j, :], offs[:, j - 1, :], tot[:, j - 1, :], ALU.add)
        pos = P.sb("pos", [128, NKT, 16], F32)
        P.tt(pos[:], pre[:], offs[:], ALU.add)
        big = P.sb("big", [128, NKT, 16], F32)
        P.ts(big[:], m[:], -BIG, ALU.mult, BIG, ALU.add)
        P.tt(pos[:], pos[:], m[:], ALU.mult)
        P.tt(pos[:], pos[:], big[:], ALU.add)
        posi = P.sb("posi", [128, NKT, 16], I32)
        P.copy(posi[:], pos[:])
        gf_ = P.sb("gf_", [128, NKT, 16], F32)
        gi = P.sb("gi", [128, NKT, 16], I32)
        P.ts(gf_[:], pos[:], float(ZROW), ALU.min)
        P.copy(gi[:], gf_[:])
        P.dma(S["gi"][:], gi[:])
        ixs = [P.sb(f"ix{i}", [128, 1], I32) for i in range(4)]
        ht = P.sb("ht", [128, 2, D], BF16)
        c = 0
        for j in range(NKT):
            b = j % 2
            P.dma(ht[:, b], S["h2"][j * 128:(j + 1) * 128, :])
            for e in range(16):
                ix = ixs[c % 4]
                c += 1
                P.copy(ix[:, :], posi[:, j, e:e + 1], eng="pool")

                def fn(eng, e=e, b=b, ix=ix):
                    return eng.indirect_dma_start(out=S["xsel"][e][:, :], out_offset=bass.IndirectOffsetOnAxis(ap=ix[:, :], axis=0),
                                                  in_=ht[:, b, :], in_offset=None,
                                                  bounds_check=P.bc_reg(eng, NSLOT - 1), oob_is_err=False)
                P.dma_custom("pool", fn, reads=[ht[:, b, :], ix[:, :]], writes=[S["xsel"][e][:, :]])
    with P.scope():
        identb = P.sb("identb", [128, 128], BF16)
        P.dma(identb[:], A["ident"][:], eng="pool")
        wg = P.sb("wg", [128, 16, FF], BF16)
        wu = P.sb("wu", [128, 16, FF], BF16)
        wd = P.sb("wd", [128, NFC, D], BF16)
        XT = P.sb("XT", [128, 16, 512], BF16)
        xr = P.sb("xr", [128, 2, D], BF16)
        hm = P.sb("hm", [128, NFC, NSLOT], BF16)
        sg = P.sb("sg", [128, 2, 512], F32)
        yt = P.sb("yt", [128, 2, D], BF16)
        pT = P.ps("pT", [128, 16, 128], BF16)
        pg = P.ps("pg", [128, 2, 512], F32)
        pu = P.ps("pu", [128, 2, 512], F32)
        pd = P.ps("pd", [128, 2, 512], F32)
        tcnt = 0
        gcnt = 0
        dcnt = 0
        for e in range(16):
            for q in range(4):
                P.dma(wg[:, q * 4:(q + 1) * 4], A["wg"][l, e, :, q * 4:(q + 1) * 4, :], eng="pool")
            for q in range(4):
                P.dma(wu[:, q * 4:(q + 1) * 4], A["wu"][l, e, :, q * 4:(q + 1) * 4, :], eng="pool")
            nq = max(1, NFC // 2)
            for q in range(0, NFC, nq):
                P.dma(wd[:, q:q + nq], A["wd"][l, e, :, q:q + nq, :], eng="pool")
            for (s0, n) in add_groups(NSLOT):
                for i in range(n // 128):
                    b = tcnt % 2
                    tcnt += 1
                    P.dma(xr[:, b], S["xsel"][e][s0 + i * 128:s0 + (i + 1) * 128, :])
                    for k in range(16):
                        P.transpose(pT[:, k, :], xr[:, b, k * 128:(k + 1) * 128], identb[:])
                    P.copy(XT[:, :, i * 128:(i + 1) * 128], pT[:], eng=("act" if tcnt % 2 else "dve"))
                for f in range(NFC):
                    pb = gcnt % 2
                    gcnt += 1
                    for k in range(16):
                        P.matmul(pg[:, pb, :n], wg[:, k, f * 128:(f + 1) * 128], XT[:, k, :n], start=(k == 0), stop=(k == 15))
                    for k in range(16):
                        P.matmul(pu[:, pb, :n], wu[:, k, f * 128:(f + 1) * 128], XT[:, k, :n], start=(k == 0), stop=(k == 15))
                    P.act(sg[:, pb, :n], pg[:, pb, :n], AF.Silu)
                    P.tt(hm[:, f, s0:s0 + n], sg[:, pb, :n], pu[:, pb, :n], ALU.mult)
            for st in range(NSLOT // 128):
                yb = st % 2
                for nb in range(4):
                    pb = dcnt % 2
                    dcnt += 1
                    for f in range(NFC):
                        P.matmul(pd[:, pb, :], hm[:, f, st * 128:(st + 1) * 128], wd[:, f, nb * 512:(nb + 1) * 512], start=(f == 0), stop=(f == NFC - 1))
                    P.copy(yt[:, yb, nb * 512:(nb + 1) * 512], pd[:, pb, :], eng=("act" if dcnt % 2 else "dve"))
                P.dma(S["yout"][e][st * 128:(st + 1) * 128, :], yt[:, yb])
    with P.scope():
        gate = P.sb("gate", [128, NKT, 16], F32)
        P.dma(gate[:], S["gate"][:])
        gi = P.sb("gi", [128, NKT, 16], I32)
        P.dma(gi[:], S["gi"][:])
        g2r = P.sb("g2r", [128, 2, D], F32)
        for j in range(2):
            P.dma(g2r[:, j], bcrow(A["modrow"][l, 2 + j:3 + j, :]))
        if last:
            fr = P.sb("fr", [128, D], F32)
            P.dma(fr[:], bcrow(A["fng_row"][0:1, :]))
            junk = P.sb("junk", [128, D], BF16)
            ss = P.sb("ss", [128, 2], F32)
        gt = P.sb("gt", [128, 3, D], BF16)
        acc = P.sb("acc", [128, 2, D], F32)
        xt = P.sb("xt", [128, 2, D], F32)
        ixs = [P.sb(f"ix{i}", [128, 1], I32) for i in range(4)]
        eps = P.eps_tile()
        c = 0
        for tl in range(2 if last else 0, NKT):
            b = tl % 2
            j = 1 if tl < 2 else 0
            rows = slice(tl * 128, (tl + 1) * 128)
            P.dma(xt[:, b], x_s[rows, :])
            for e in range(16):
                ix = ixs[c % 4]
                g = c % 3
                c += 1
                P.copy(ix[:, :], gi[:, tl, e:e + 1], eng="pool")

                def fn2(eng, e=e, g=g, ix=ix):
                    return eng.indirect_dma_start(out=gt[:, g, :], out_offset=None, in_=S["yout"][e][:, :],
                                                  in_offset=bass.IndirectOffsetOnAxis(ap=ix[:, :], axis=0))
                P.dma_custom("pool", fn2, reads=[S["yout"][e][:, :], ix[:, :]], writes=[gt[:, g, :]])
                if e == 0:
                    P.ts(acc[:, b], gt[:, g, :], gate[:, tl, 0:1], ALU.mult)
                else:
                    P.stt(acc[:, b], gt[:, g, :], gate[:, tl, e:e + 1], acc[:, b], ALU.mult, ALU.add)
            P.tt(acc[:, b], acc[:, b], g2r[:, j], ALU.mult)
            P.tt(acc[:, b], acc[:, b], xt[:, b], ALU.add)
            if not last:
                P.dma(x_s[rows, :], acc[:, b])
            else:
                P.act(junk[:], acc[:, b], AF.Square, accum_out=ss[:, b:b + 1])
                P.act(ss[:, b:b + 1], ss[:, b:b + 1], AF.Sqrt, bias=eps[:, :], scale=1.0 / D)
                P.recip(ss[:, b:b + 1], ss[:, b:b + 1])
                P.ts(acc[:, b], acc[:, b], ss[:, b:b + 1], ALU.mult)
                P.tt(acc[:, b], acc[:, b], fr[:], ALU.mult)
                P.dma(out_d[(tl - 2) * 128:(tl - 1) * 128, :], acc[:, b])


def phase_mod(P, A, DEPTH):
    with P.scope():
        identf = P.sb("identf", [128, 128], F32)
        P.dma(identf[:], A["ident"][:])
        c_sb = P.sb("c_sb", [128, 16, 2], F32)
        s_sb = P.sb("s_sb", [128, 16, 2], F32)
        P.dma(c_sb[:], A["cT2"][:])
        P.act(s_sb[:], c_sb[:], AF.Silu)
        wbuf = P.sb("wbuf", [128, 2, 16, 512], F32)
        bsb = P.sb("bsb", [2, 6 * D], F32)
        msb = P.sb("msb", [2, 6 * D], F32)
        mfm = P.sb("mfm", [128, 16, 8], F32)
        ps = P.ps("ps", [2, 2, 512], F32)
        pm = P.ps("pm", [128, 16, 2, 2], F32)
        P.memset(mfm[:], 0.0)
        i = 0
        for l in range(DEPTH):
            P.dma(bsb[:], A["bm"][l])
            for nb in range(6 * D // 512):
                buf = i % 2
                i += 1
                P.dma(wbuf[:, buf], A["wm"][l, :, :, nb * 512:(nb + 1) * 512])
                for k in range(16):
                    P.matmul(ps[:, buf, :], s_sb[:, k, :], wbuf[:, buf, k, :], start=(k == 0), stop=(k == 15))
                P.tt(msb[:, nb * 512:(nb + 1) * 512], ps[:, buf, :], bsb[:, nb * 512:(nb + 1) * 512], ALU.add)
            for idx, (r, seg) in enumerate([(0, 2), (1, 2), (0, 5), (1, 5), (0, 4), (0, 3), (1, 4), (1, 3)]):
                P.dma(A["modrow"][l, idx:idx + 1, :], msb[r:r + 1, seg * D:(seg + 1) * D])
            for v in range(2):
                for k in range(16):
                    P.transpose(pm[:, k, v, :], msb[0:2, v * D + k * 128:v * D + (k + 1) * 128], identf[0:2, 0:2])
            P.copy(mfm[:, :, 0], pm[:, :, 0, 0])
            P.copy(mfm[:, :, 1], pm[:, :, 1, 0])
            P.copy(mfm[:, :, 2], pm[:, :, 0, 1])
            P.copy(mfm[:, :, 3], pm[:, :, 1, 1])
            P.dma(A["modfm"][l], mfm[:])


def build_main(NL=SEQ, FF=1024, DEPTH=2, upto=None, dbg=False, final=True):
    P = Prog()
    NT = CTX + NL
    NKT = NT // 128
    KL = NL // 8
    LCAP = KL + 128
    NSLOT = LCAP + 128
    A = {}

    def inp(name, shape, dt=F32):
        A[name] = P.dram(name, shape, dt, "ExternalInput")

    inp("x_in", [NT, D])
    inp("cT2", [128, 16, 2])
    inp("wm", [DEPTH, 128, 16, 6 * D])
    inp("bm", [DEPTH, 2, 6 * D])
    A["modfm"] = P.dram("s_modfm", [DEPTH, 128, 16, 8], F32)
    A["modrow"] = P.dram("s_modrow", [DEPTH, 8, D], F32)
    inp("n1g", [DEPTH, 128, 16])
    inp("n2g_row", [DEPTH, 1, D])
    inp("ong", [DEPTH, 128, 16])
    inp("fng_row", [1, D])
    inp("w_fm", [DEPTH, FMW // 128, 128, 16 * 128])
    inp("w_tm", [DEPTH, TMW // 256, 128, 16 * 256])
    inp("kvg", [DEPTH, 128, 2])
    inp("qg", [DEPTH, 128, 4])
    inp("wuk", [DEPTH, 128, 2, 8, 128])
    inp("wuv", [DEPTH, 128, 2, 8, 128])
    inp("wqn", [DEPTH, 128, 4, 8, 128])
    inp("wqr", [DEPTH, 128, 4, 8, 2, 64])
    inp("cosT", [64, NL])
    inp("sinT", [64, NL])
    inp("natb", [DEPTH, 4, 5, 128, 576])
    inp("ident", [128, 128])
    inp("dec", [DEPTH, 128, 8])
    inp("retE", [2, 128, 128])
    inp("retM", [2, 128, 128])
    inp("retwk", [128, 2])
    inp("retwq", [128, 2, 128])
    inp("w_out", [DEPTH, 128, 16, D])
    inp("w_r", [DEPTH, 128, 16, 16])
    inp("wg", [DEPTH, 16, 128, 16, FF])
    inp("wu", [DEPTH, 16, 128, 16, FF])
    inp("wd", [DEPTH, 16, 128, FF // 128, D])
    inp("tri", [128, 128])
    out_d = P.dram("out", [NL, D], F32, "ExternalOutput") if final else None
    S = {
        "cqT": P.dram("s_cqT", [512, NT], F32), "ckvT": P.dram("s_ckvT", [256, NT], F32),
        "krT2": P.dram("s_krT2", [128, NT], F32),
        "nqT": P.dram("s_nqT", [512, NT], BF16), "nkT": P.dram("s_nkT", [512, NT], BF16),
        "rqT": P.dram("s_rqT", [512, NT], BF16), "rkT": P.dram("s_rkT", [512, NT], BF16),
        "nv": P.dram("s_nv", [NT, 512], BF16), "rk": P.dram("s_rk", [NT, 512], BF16),
        "rv": P.dram("s_rv", [NT, 512], BF16), "gfb": P.dram("s_gfb", [NT, 1024], F32),
        "mix": P.dram("s_mix", [NT, D], F32), "h2": P.dram("s_h2", [NT, D], BF16),
        "aff": P.dram("s_aff", [NT, 16], F32), "gate": P.dram("s_gate", [128, NKT, 16], F32),
        "gi": P.dram("s_gi", [128, NKT, 16], I32),
        "xsel": [P.dram(f"s_xsel{e}", [NSLOT, D], BF16) for e in range(16)],
        "yout": [P.dram(f"s_yout{e}", [NSLOT + 128, D], BF16) for e in range(16)],
    }
    x_s = P.dram("s_x", [NT, D], F32, "ExternalOutput" if (dbg or not final) else "Internal")
    if dbg:
        for k_ in ("mix", "aff", "cqT", "gfb"):
            sh_, dt_ = {"mix": ([NT, D], F32), "aff": ([NT, 16], F32), "cqT": ([512, NT], F32), "gfb": ([NT, 1024], F32)}[k_]
            S[k_] = P.dram("d_" + k_, sh_, dt_, "ExternalOutput")
    with P.scope():
        xt = P.sb("xt", [128, 2, D], F32)
        for tl in range(NKT):
            P.dma(xt[:, tl % 2], A["x_in"][tl * 128:(tl + 1) * 128, :])
            P.dma(x_s[tl * 128:(tl + 1) * 128, :], xt[:, tl % 2])
        zb = P.sb("zb", [128, D], BF16)
        zf = P.sb("zf", [128, D], F32)
        P.memset(zb[:], 0.0)
        P.memset(zf[:], 0.0)
        for e in range(16):
            for st in range(NSLOT // 128):
                P.dma(S["xsel"][e][st * 128:(st + 1) * 128, :], zb[:])
            P.dma(S["yout"][e][NSLOT:NSLOT + 128, :], zb[:])
    phase_mod(P, A, DEPTH)
    nph = 0
    for l in range(DEPTH):
        phases = [
            lambda: phase_proj2(P, NT, x_s, S, A, l),
            lambda: phase_mla(P, NT, S["cqT"], S["ckvT"], S["krT2"], S["mix"], A, l, None),
            lambda: phase_nat(P, NT, S["nqT"], S["nkT"], S["nv"], S["mix"], A["natb"], l, A["ident"]),
            lambda: phase_ret(P, NT, S["rqT"], S["rkT"], S["rk"], S["rv"], S["gfb"], S["mix"], A, l),
            lambda: phase_post(P, NT, x_s, S, A, l),
            lambda: phase_moe(P, NT, x_s, S, A, l, FF, final and l == DEPTH - 1, out_d),
        ]
        for ph in phases:
            if upto is not None and nph >= upto:
                break
            ph()
            nph += 1
    nc = P.finish()
    return nc, P


def kchunk(w):
    K, N = w.shape
    return np.ascontiguousarray(w.reshape(K // 128, 128, N).transpose(1, 0, 2))


def prep_shared(inp, NL, DEPTH):
    A = {}
    w_in = inp["w_in"]
    fm_l, tm_l = [], []
    for l in range(DEPTH):
        w = w_in[l]
        kr = w[:, 768:832]
        fm = np.concatenate([w[:, 0:512], w[:, 512:768], kr, kr[:, SW64], w[:, 832:1344], w[:, 1344:1856],
                             w[:, 2368:2880], w[:, 2880:3392]], 1)
        tm = np.concatenate([w[:, 1856:2368], w[:, 2880:3392], w[:, 3392:3904], w[:, 3904:4416], w[:, 4416:4928]], 1)
        fm_l.append(np.ascontiguousarray(kchunk(fm).reshape(128, 16, FMW // 128, 128).transpose(2, 0, 1, 3)).reshape(FMW // 128, 128, 16 * 128))
        tm_l.append(np.ascontiguousarray(kchunk(tm).reshape(128, 16, TMW // 256, 256).transpose(2, 0, 1, 3)).reshape(TMW // 256, 128, 16 * 256))
    A["w_fm"] = np.stack(fm_l)
    A["w_tm"] = np.stack(tm_l)
    A["n1g"] = np.stack([fm16(inp["norm1_g"][l]) for l in range(DEPTH)])
    A["ong"] = np.stack([fm16(inp["out_norm_g"][l]) for l in range(DEPTH)])
    A["n2g_row"] = np.ascontiguousarray(inp["norm2_g"][:DEPTH, None, :])
    A["fng_row"] = np.ascontiguousarray(inp["final_norm_g"][None, :])
    A.update(mla_weights(inp["mla_q_norm_g"][:DEPTH], inp["mla_kv_norm_g"][:DEPTH], inp["mla_w_uq"][:DEPTH],
                         inp["mla_w_uk"][:DEPTH], inp["mla_w_uv"][:DEPTH]))
    A["cosT"], A["sinT"] = rope_tables_T(NL)
    A["natb"] = np.stack([nat_bias_tables(inp["nat_rpb"][l]) for l in range(DEPTH)])
    A["ident"] = np.eye(128, dtype=np.float32)
    dec = np.concatenate([inp["ret_decay_f"][:DEPTH], inp["ret_decay_b"][:DEPTH]], 1)
    A["dec"] = np.ascontiguousarray(np.broadcast_to(dec[:, None, :], (DEPTH, 128, 8))).astype(np.float32)
    A.update(ret_consts())
    A["w_out"] = np.stack([kchunk(inp["w_out"][l]) for l in range(DEPTH)])
    A["w_r"] = np.stack([kchunk(inp["w_router"][l]) for l in range(DEPTH)])
    A["wg"] = np.stack([np.stack([kchunk(inp["w_gate"][l, e]) for e in range(16)]) for l in range(DEPTH)])
    A["wu"] = np.stack([np.stack([kchunk(inp["w_up"][l, e]) for e in range(16)]) for l in range(DEPTH)])
    A["wd"] = np.stack([np.stack([kchunk(inp["w_down"][l, e]) for e in range(16)]) for l in range(DEPTH)])
    A["tri"] = (np.arange(128)[:, None] < np.arange(128)[None, :]).astype(np.float32)
    return A


def prep_sample(x_b, ctx_b, c_b, c_ctx):
    A = {}
    A["x_in"] = np.ascontiguousarray(np.concatenate([ctx_b, x_b], 0))
    C2 = np.stack([c_b, c_ctx], 0)
    A["cT2"] = np.ascontiguousarray(C2.reshape(2, 16, 128).transpose(2, 1, 0))
    return A


def kernel(x, c, ctx, c_ctx, w_mod, b_mod, **rest):
    inp = {k: np.asarray(v, dtype=np.float32) for k, v in rest.items()}
    x = np.asarray(x, np.float32)
    ctx = np.asarray(ctx, np.float32)
    c = np.asarray(c, np.float32)
    c_ctx = np.asarray(c_ctx, np.float32)
    w_mod = np.asarray(w_mod, np.float32)
    b_mod = np.asarray(b_mod, np.float32)
    shared = prep_shared(inp, SEQ, DEPTH)
    shared["wm"] = np.stack([kchunk(w_mod[l]) for l in range(DEPTH)])
    shared["bm"] = np.ascontiguousarray(np.broadcast_to(b_mod[:DEPTH, None, :], (DEPTH, 2, 6 * D))).astype(np.float32)
    nc, _ = build_main(SEQ, 1024, DEPTH)
    in_maps = []
    for b in range(NB):
        m = dict(shared)
        m.update(prep_sample(x[b], ctx[b], c[b], c_ctx))
        in_maps.append(m)
    res = run_bass_kernel_spmd(nc, in_maps, core_ids=list(range(NB)))
    return np.stack([res.results[b]["out"] for b in range(NB)]).astype(np.float32)
```
